# Optimizing a Trainium2 kernel written in Bass

```python
import math
import numpy as np
import jax
import jax.numpy as jnp
from jax import lax

D_MODEL = 1024
BATCH = 8
SEQ = 2048
DEPTH = 4

CTX_LEN = 256
GRID_W = 64
N_MIXERS = 3
EXPAND = 2
D_INNER = EXPAND * D_MODEL
EPS = 1e-6
DIFF_DH = 64
DIFF_VDH = 2 * DIFF_DH
DIFF_HEADS = D_INNER // DIFF_VDH
ATTN_QBLOCK = 128
ROPE_THETA = 10000.0
NA_DH = 64
NA_HEADS = D_INNER // NA_DH
NA_WIN_H = 8
NA_WIN_W = 16
NA_QCB = NA_WIN_W
NA_BAND = 2 * NA_WIN_W
NA_NCB = GRID_W // NA_QCB
MLSTM_HEADS = 4
MLSTM_DH = D_INNER // MLSTM_HEADS
MLSTM_CONV = 4
MLSTM_CONV_PAD = (2, 1)
QKV_BLOCK = 4
QKV_NBLK = D_INNER // QKV_BLOCK
MLSTM_CHUNK = 64

kernel_name = "hybrid_diffattn_natten_mlstm_prefix_dit"

F32 = jnp.float32


def rms_norm(x, g):
    x32 = x.astype(F32)
    y = x32 * lax.rsqrt(jnp.mean(x32 * x32, axis=-1, keepdims=True) + EPS)
    return (y * g.astype(F32)).astype(x.dtype)


def adaln(cvec, w, b):
    e = jax.nn.silu(cvec) @ w + b
    return jnp.split(e, 3, axis=-1)


def axial_rope(n_tok, dtype):
    t = jnp.arange(n_tok)
    row = (t // GRID_W).astype(F32)
    col = (t % GRID_W).astype(F32)
    n_freq = DIFF_DH // 4
    inv = ROPE_THETA ** (-jnp.arange(n_freq, dtype=F32) / n_freq)
    ar = row[:, None] * inv
    ac = col[:, None] * inv
    ang = jnp.concatenate([ar, ar, ac, ac], axis=-1)[:, None, :]
    return jnp.cos(ang).astype(dtype), jnp.sin(ang).astype(dtype)


def apply_axial_rope(x, cos, sin):
    xs = x.reshape(*x.shape[:-1], 2, 2, DIFF_DH // 4)
    rot = jnp.concatenate([-xs[..., 1:, :], xs[..., :1, :]], axis=-2).reshape(x.shape)
    return x * cos + rot * sin


def diff_softmax_pair(q, k, v, lam, scale):
    s = jnp.einsum('bqnd,bknd->bnqk', q, k).astype(F32) * scale
    p = jax.nn.softmax(s, axis=-1)
    bsz, n2, nq, nk = p.shape
    p = p.reshape(bsz, n2 // 2, 2, nq, nk)
    w = p[:, :, 0] - lam * p[:, :, 1]
    return jnp.einsum('bhqk,bkhe->bqhe', w.astype(v.dtype), v)


def diff_attention_mixer(u_lat, u_ctx, w_in, w_out, qn_g, kn_g, lam_vecs, subln_g, layer_idx, need_ctx):
    bsz, n_lat, _ = u_lat.shape
    n_ctx = u_ctx.shape[1]

    def project(u, n):
        q, k, v, z = jnp.split(u @ w_in, 4, axis=-1)
        q = rms_norm(q.reshape(bsz, n, 2 * DIFF_HEADS, DIFF_DH), qn_g)
        k = rms_norm(k.reshape(bsz, n, 2 * DIFF_HEADS, DIFF_DH), kn_g)
        v = v.reshape(bsz, n, DIFF_HEADS, DIFF_VDH)
        return q, k, v, z

    q_l, k_l, v_l, z_l = project(u_lat, n_lat)
    q_c, k_c, v_c, z_c = project(u_ctx, n_ctx)
    cos, sin = axial_rope(n_lat, q_l.dtype)
    q_l = apply_axial_rope(q_l, cos, sin)
    k_l = apply_axial_rope(k_l, cos, sin)
    k_all = jnp.concatenate([k_l, k_c], axis=1)
    v_all = jnp.concatenate([v_l, v_c], axis=1)

    lam_init = 0.8 - 0.6 * math.exp(-0.3 * layer_idx)
    lv = lam_vecs.astype(F32)
    lam = jnp.exp(jnp.sum(lv[0] * lv[1])) - jnp.exp(jnp.sum(lv[2] * lv[3])) + lam_init
    scale = DIFF_DH ** -0.5

    n_blocks = n_lat // ATTN_QBLOCK
    q_blocks = jnp.moveaxis(q_l.reshape(bsz, n_blocks, ATTN_QBLOCK, 2 * DIFF_HEADS, DIFF_DH), 1, 0)
    o_l = lax.map(lambda qb: diff_softmax_pair(qb, k_all, v_all, lam, scale), q_blocks)
    o_l = jnp.moveaxis(o_l, 0, 1).reshape(bsz, n_lat, DIFF_HEADS, DIFF_VDH)

    def finish(o, z, n):
        o = rms_norm(o, subln_g) * (1.0 - lam_init)
        return (o.reshape(bsz, n, D_INNER) * jax.nn.silu(z)) @ w_out

    out_l = finish(o_l, z_l, n_lat)
    out_c = finish(diff_softmax_pair(q_c, k_c, v_c, lam, scale), z_c, n_ctx) if need_ctx else None
    return out_l, out_c


def neighbourhood_mixer(u_lat, u_ctx, w_in, w_out, qn_g, kn_g, rpb, need_ctx):
    bsz, n_lat, _ = u_lat.shape
    n_ctx = u_ctx.shape[1]
    rows = n_lat // GRID_W
    kh = min(NA_WIN_H, rows)
    scale = NA_DH ** -0.5

    def project(u, n):
        q, k, v, z = jnp.split(u @ w_in, 4, axis=-1)
        q = rms_norm(q.reshape(bsz, n, NA_HEADS, NA_DH), qn_g)
        k = rms_norm(k.reshape(bsz, n, NA_HEADS, NA_DH), kn_g)
        v = v.reshape(bsz, n, NA_HEADS, NA_DH)
        return q, k, v, z

    q_l, k_l, v_l, z_l = project(u_lat, n_lat)
    q_c, k_c, v_c, z_c = project(u_ctx, n_ctx)
    dt = v_l.dtype

    cols = np.arange(GRID_W)
    win_start = np.clip(cols - NA_WIN_W // 2, 0, GRID_W - NA_WIN_W)
    band_start = np.clip(np.arange(NA_NCB) * NA_QCB - NA_WIN_W // 2, 0, GRID_W - NA_BAND)
    band_cols = band_start[:, None] + np.arange(NA_BAND)
    qcols = np.arange(NA_NCB)[:, None] * NA_QCB + np.arange(NA_QCB)
    kc = band_cols[:, None, :]
    qc = qcols[:, :, None]
    valid = jnp.asarray((kc >= win_start[qc]) & (kc < win_start[qc] + NA_WIN_W))
    col_bias_idx = np.clip(kc - qc + NA_WIN_W - 1, 0, 2 * NA_WIN_W - 2)
    row_start = jnp.asarray(np.clip(np.arange(rows) - kh // 2, 0, rows - kh).astype(np.int32))

    k_grid = k_l.reshape(bsz, rows, GRID_W, NA_HEADS, NA_DH)
    v_grid = v_l.reshape(bsz, rows, GRID_W, NA_HEADS, NA_DH)
    q_rows = jnp.moveaxis(q_l.reshape(bsz, rows, NA_NCB, NA_QCB, NA_HEADS, NA_DH), 1, 0)
    n_win = kh * NA_BAND

    def row_step(args):
        q_r, r, rs = args
        kr = lax.dynamic_slice_in_dim(k_grid, rs, kh, axis=1)
        vr = lax.dynamic_slice_in_dim(v_grid, rs, kh, axis=1)
        k_band = kr[:, :, band_cols]
        v_band = vr[:, :, band_cols]
        s = jnp.einsum('bnqhd,binxhd->bhnqix', q_r, k_band).astype(F32) * scale
        row_idx = rs - r + jnp.arange(kh) + (NA_WIN_H - 1)
        bias = rpb[:, row_idx][:, :, col_bias_idx]
        bias = jnp.transpose(bias, (0, 2, 3, 1, 4)).astype(F32)
        s = jnp.where(valid[:, :, None, :], s + bias, -jnp.inf)
        s = s.reshape(bsz, NA_HEADS, NA_NCB, NA_QCB, n_win)
        s_ctx = jnp.einsum('bnqhd,bkhd->bhnqk', q_r, k_c).astype(F32) * scale
        p = jax.nn.softmax(jnp.concatenate([s, s_ctx], axis=-1), axis=-1)
        p_band = p[..., :n_win].reshape(bsz, NA_HEADS, NA_NCB, NA_QCB, kh, NA_BAND).astype(dt)
        p_ctx = p[..., n_win:].astype(dt)
        return (jnp.einsum('bhnqix,binxhd->bnqhd', p_band, v_band)
                + jnp.einsum('bhnqk,bkhd->bnqhd', p_ctx, v_c))

    o_l = lax.map(row_step, (q_rows, jnp.arange(rows, dtype=jnp.int32), row_start))
    o_l = jnp.moveaxis(o_l, 0, 1).reshape(bsz, n_lat, D_INNER)
    out_l = (o_l * jax.nn.silu(z_l)) @ w_out
    out_c = None
    if need_ctx:
        s = jnp.einsum('bqhd,bkhd->bhqk', q_c, k_c).astype(F32) * scale
        p = jax.nn.softmax(s, axis=-1).astype(dt)
        o_c = jnp.einsum('bhqk,bkhd->bqhd', p, v_c).reshape(bsz, n_ctx, D_INNER)
        out_c = (o_c * jax.nn.silu(z_c)) @ w_out
    return out_l, out_c


def mlstm_chunk_scan(q, k, v, i_pre, log_f, state):
    bsz, nh, n_tok, dh = q.shape
    L = MLSTM_CHUNK
    nc = n_tok // L

    def to_chunks(t):
        return jnp.moveaxis(t.reshape(bsz, nh, nc, L, *t.shape[3:]), 2, 0)

    causal = jnp.tril(jnp.ones((L, L), dtype=bool))

    def step(carry, xs):
        C, n, m = carry
        qc, kc, vc, ic, fc = xs
        b = jnp.cumsum(fc, axis=-1)
        dmat = jnp.where(causal, b[..., :, None] - b[..., None, :] + ic[..., None, :], -jnp.inf)
        g = b + m[..., None]
        m_t = jnp.maximum(g, jnp.max(dmat, axis=-1))
        wts = jnp.exp(dmat - m_t[..., None]) * jnp.einsum('bhtd,bhsd->bhts', qc, kc)
        inter = jnp.exp(g - m_t)
        num = jnp.einsum('bhts,bhsd->bhtd', wts, vc) + inter[..., None] * jnp.einsum('bhtd,bhde->bhte', qc, C)
        den = jnp.sum(wts, axis=-1) + inter * jnp.einsum('bhtd,bhd->bht', qc, n)
        h = num / jnp.maximum(jnp.abs(den), jnp.exp(-m_t))[..., None]
        m_new = m_t[..., -1]
        decay = jnp.exp(b[..., -1:] - b + ic - m_new[..., None])
        carry_scale = jnp.exp(b[..., -1] + m - m_new)
        C_new = carry_scale[..., None, None] * C + jnp.einsum('bhs,bhsd,bhse->bhde', decay, kc, vc)
        n_new = carry_scale[..., None] * n + jnp.einsum('bhs,bhsd->bhd', decay, kc)
        return (C_new, n_new, m_new), h

    final, hs = lax.scan(step, state, (to_chunks(q), to_chunks(k), to_chunks(v),
                                       to_chunks(i_pre), to_chunks(log_f)))
    h = jnp.moveaxis(hs, 0, 2).reshape(bsz, nh, n_tok, dh)
    return h, final


def mlstm_mixer(u_lat, u_ctx, w_in, conv_w, conv_b, wq, wk, wv, gate_w, gate_b, mhn_g, skip, w_out, need_ctx):
    bsz = u_lat.shape[0]
    dt = u_lat.dtype

    def dwconv(t):
        y = lax.conv_general_dilated(t, conv_w[:, None, :], window_strides=(1,), padding=[MLSTM_CONV_PAD],
                                     dimension_numbers=('NWC', 'WIO', 'NWC'), feature_group_count=D_INNER)
        return y + conv_b

    def headwise(t, w):
        tb = t.reshape(*t.shape[:-1], QKV_NBLK, QKV_BLOCK)
        return jnp.einsum('btnd,nde->btne', tb, w).reshape(t.shape)

    def to_heads(t):
        n = t.shape[1]
        return jnp.transpose(t.reshape(bsz, n, MLSTM_HEADS, MLSTM_DH), (0, 2, 1, 3)).astype(F32)

    def branch(u):
        xm, z, og = jnp.split(u @ w_in, 3, axis=-1)
        xc = jax.nn.silu(dwconv(xm))
        q = headwise(xc, wq)
        k = headwise(xc, wk)
        v = headwise(xm, wv)
        gin = jnp.concatenate([q, k, v], axis=-1).astype(F32)
        gates = []
        for d in range(2):
            pre = gin @ gate_w[d].astype(F32) + gate_b[d].astype(F32)
            i_pre = jnp.transpose(pre[..., :MLSTM_HEADS], (0, 2, 1))
            log_f = jax.nn.log_sigmoid(jnp.transpose(pre[..., MLSTM_HEADS:], (0, 2, 1)))
            gates.append((i_pre, log_f))
        qkv = (to_heads(q), to_heads(k) * (MLSTM_DH ** -0.5), to_heads(v))
        return xc, z, og, qkv, gates

    xc_l, z_l, og_l, qkv_l, gates_l = branch(u_lat)
    xc_c, z_c, og_c, qkv_c, gates_c = branch(u_ctx)

    zero = (jnp.zeros((bsz, MLSTM_HEADS, MLSTM_DH, MLSTM_DH), F32),
            jnp.zeros((bsz, MLSTM_HEADS, MLSTM_DH), F32),
            jnp.zeros((bsz, MLSTM_HEADS), F32))
    flip = lambda t: jnp.flip(t, axis=2)
    hf_c, st_f = mlstm_chunk_scan(*qkv_c, *gates_c[0], zero)
    hf_l, _ = mlstm_chunk_scan(*qkv_l, *gates_l[0], st_f)
    hb_c, st_b = mlstm_chunk_scan(*[flip(t) for t in qkv_c], *[flip(t) for t in gates_c[1]], zero)
    hb_l, _ = mlstm_chunk_scan(*[flip(t) for t in qkv_l], *[flip(t) for t in gates_l[1]], st_b)

    def finish(h, og, xc, z):
        h = jnp.transpose(h, (0, 2, 1, 3))
        n = h.shape[1]
        h = h * jax.nn.sigmoid(og.astype(F32)).reshape(h.shape)
        mu = jnp.mean(h, axis=-1, keepdims=True)
        var = jnp.mean(jnp.square(h - mu), axis=-1, keepdims=True)
        hn = ((h - mu) * lax.rsqrt(var + EPS)).reshape(bsz, n, D_INNER) * mhn_g.astype(F32)
        y = (hn + skip.astype(F32) * xc.astype(F32)) * jax.nn.silu(z.astype(F32))
        return y.astype(dt) @ w_out

    out_l = finish(hf_l + flip(hb_l), og_l, xc_l, z_l)
    out_c = finish(hf_c + flip(hb_c), og_c, xc_c, z_c) if need_ctx else None
    return out_l, out_c


def setup_inputs(seed: int = 0) -> dict:
    key = jax.random.key(seed)
    ks = iter(jax.random.split(key, 48))

    def nrm(shape, s):
        return jax.random.normal(next(ks), shape, F32) * s

    D, E = D_MODEL, D_INNER
    n_a = len(range(0, DEPTH, N_MIXERS))
    n_b = len(range(1, DEPTH, N_MIXERS))
    n_c = len(range(2, DEPTH, N_MIXERS))
    f_bias = jnp.linspace(3.0, 6.0, MLSTM_HEADS, dtype=F32) + nrm((n_c, 2, MLSTM_HEADS), 0.1)
    i_bias = nrm((n_c, 2, MLSTM_HEADS), 0.1)
    return {
        "x": nrm((BATCH, SEQ, D), 1.0),
        "c": nrm((BATCH, D), 1.0),
        "ctx": nrm((BATCH, CTX_LEN, D), 1.0),
        "c_ctx": nrm((D,), 1.0),
        "mod_w": nrm((DEPTH, D, 3 * D), 0.5 * D ** -0.5),
        "mod_b": nrm((DEPTH, 3 * D), 0.02),
        "norm_g": 1.0 + nrm((DEPTH, D), 0.02),
        "a_w_in": nrm((n_a, D, 4 * E), D ** -0.5),
        "a_w_out": nrm((n_a, E, D), E ** -0.5),
        "a_qn_g": 1.0 + nrm((n_a, DIFF_DH), 0.02),
        "a_kn_g": 1.0 + nrm((n_a, DIFF_DH), 0.02),
        "a_lam": nrm((n_a, 4, DIFF_DH), 0.1),
        "a_subln_g": 1.0 + nrm((n_a, DIFF_VDH), 0.02),
        "b_w_in": nrm((n_b, D, 4 * E), D ** -0.5),
        "b_w_out": nrm((n_b, E, D), E ** -0.5),
        "b_qn_g": 1.0 + nrm((n_b, NA_DH), 0.02),
        "b_kn_g": 1.0 + nrm((n_b, NA_DH), 0.02),
        "b_rpb": nrm((n_b, NA_HEADS, 2 * NA_WIN_H - 1, 2 * NA_WIN_W - 1), 0.1),
        "c_w_in": nrm((n_c, D, 3 * E), D ** -0.5),
        "c_conv_w": nrm((n_c, MLSTM_CONV, E), MLSTM_CONV ** -0.5),
        "c_conv_b": nrm((n_c, E), 0.02),
        "c_wq": nrm((n_c, QKV_NBLK, QKV_BLOCK, QKV_BLOCK), QKV_BLOCK ** -0.5),
        "c_wk": nrm((n_c, QKV_NBLK, QKV_BLOCK, QKV_BLOCK), QKV_BLOCK ** -0.5),
        "c_wv": nrm((n_c, QKV_NBLK, QKV_BLOCK, QKV_BLOCK), QKV_BLOCK ** -0.5),
        "c_gate_w": nrm((n_c, 2, 3 * E, 2 * MLSTM_HEADS), (3 * E) ** -0.5),
        "c_gate_b": jnp.concatenate([i_bias, f_bias], axis=-1),
        "c_mhn_g": 1.0 + nrm((n_c, E), 0.02),
        "c_skip": 1.0 + nrm((n_c, E), 0.02),
        "c_w_out": nrm((n_c, E, D), E ** -0.5),
    }


def reference(x, c, ctx, c_ctx, mod_w, mod_b, norm_g,
              a_w_in, a_w_out, a_qn_g, a_kn_g, a_lam, a_subln_g,
              b_w_in, b_w_out, b_qn_g, b_kn_g, b_rpb,
              c_w_in, c_conv_w, c_conv_b, c_wq, c_wk, c_wv, c_gate_w, c_gate_b, c_mhn_g, c_skip, c_w_out):
    h_lat = x
    h_ctx = ctx
    for i in range(DEPTH):
        kind = i % N_MIXERS
        j = i // N_MIXERS
        need_ctx = i < DEPTH - 1
        shift, scale, gate = adaln(c, mod_w[i], mod_b[i])
        shift_c, scale_c, gate_c = adaln(c_ctx, mod_w[i], mod_b[i])
        u_lat = rms_norm(h_lat, norm_g[i]) * (1.0 + scale[:, None, :]) + shift[:, None, :]
        u_ctx = rms_norm(h_ctx, norm_g[i]) * (1.0 + scale_c) + shift_c
        if kind == 0:
            out_l, out_c = diff_attention_mixer(u_lat, u_ctx, a_w_in[j], a_w_out[j], a_qn_g[j], a_kn_g[j],
                                                a_lam[j], a_subln_g[j], i, need_ctx)
        elif kind == 1:
            out_l, out_c = neighbourhood_mixer(u_lat, u_ctx, b_w_in[j], b_w_out[j], b_qn_g[j], b_kn_g[j],
                                               b_rpb[j], need_ctx)
        else:
            out_l, out_c = mlstm_mixer(u_lat, u_ctx, c_w_in[j], c_conv_w[j], c_conv_b[j], c_wq[j], c_wk[j],
                                       c_wv[j], c_gate_w[j], c_gate_b[j], c_mhn_g[j], c_skip[j], c_w_out[j],
                                       need_ctx)
        h_lat = h_lat + gate[:, None, :] * out_l
        if need_ctx:
            h_ctx = h_ctx + gate_c * out_c
    return h_lat
```

```python
import math
import os
from contextlib import ExitStack

import numpy as np
import concourse.bass as bass
import concourse.mybir as mybir
from concourse.bass_utils import run_bass_kernel_spmd

F32 = mybir.dt.float32
BF16 = mybir.dt.bfloat16
AF = mybir.ActivationFunctionType
ALU = mybir.AluOpType
AX = mybir.AxisListType

EPOCH = 8000
DMA_EPOCH = 400


class Buf:
    __slots__ = ("name", "lw", "rd")

    def __init__(self, name=""):
        self.name = name
        self.lw = None
        self.rd = {}


class K:
    def __init__(self, nc, es):
        self.nc = nc
        self.es = es
        self.eng = {"pe": nc.tensor, "act": nc.scalar, "dve": nc.vector, "pool": nc.gpsimd, "sp": nc.sync}
        self.sem = {}
        self.unit = {}
        self.cnt = {}
        self.known = {e: {} for e in self.eng}
        self.vc = {}
        self.cur = {}
        self.epoch_n = {e: 0 for e in self.eng}
        self.dma_rr = {"sp": 0, "pool": 0}
        self.dma_tracks = {"sp": [], "pool": []}
        self.n_inst = 0
        for e in ("pe", "act", "dve", "pool"):
            self._new_compute_track(e)
        for i in range(16):
            self._new_dma_track("sp")
        for i in range(8):
            self._new_dma_track("pool")

    def _new_sem(self, name):
        return self.es.enter_context(self.nc.semaphore(name))

    def _new_compute_track(self, e):
        name = f"{e}{self.epoch_n[e]}"
        self.epoch_n[e] += 1
        self.sem[name] = self._new_sem("s_" + name)
        self.unit[name] = 1
        self.cnt[name] = 0
        self.cur[e] = name

    def _new_dma_track(self, q):
        name = f"dma{q}{len(self.sem)}"
        self.sem[name] = self._new_sem("s_" + name)
        self.unit[name] = 16
        self.cnt[name] = 0
        self.dma_tracks[q].append(name)
        return name

    def _deps(self, reads, writes):
        deps = {}
        for b in reads:
            if b.lw is not None:
                t, v = b.lw
                if deps.get(t, 0) < v:
                    deps[t] = v
        for b in writes:
            if b.lw is not None:
                t, v = b.lw
                if deps.get(t, 0) < v:
                    deps[t] = v
            for t, v in b.rd.items():
                if deps.get(t, 0) < v:
                    deps[t] = v
        return deps

    def _wait(self, e, deps, skip_track=None):
        kn = self.known[e]
        for t, v in deps.items():
            if t == skip_track:
                continue
            if kn.get(t, 0) >= v:
                continue
            if self.unit[t] == 16:
                v = self.cnt[t]
            self.eng[e].wait_ge(self.sem[t], v * self.unit[t])
            snap = self.vc.get((t, v))
            if snap:
                for t2, v2 in snap.items():
                    if kn.get(t2, 0) < v2:
                        kn[t2] = v2
            kn[t] = max(kn.get(t, 0), v)

    def _commit(self, e, inst, track, reads, writes):
        self.cnt[track] += 1
        v = self.cnt[track]
        inst.then_inc(self.sem[track], self.unit[track])
        snap = dict(self.known[e])
        self.vc[(track, v)] = snap
        for b in writes:
            b.lw = (track, v)
            b.rd = {}
        for b in reads:
            if b.rd.get(track, 0) < v:
                b.rd[track] = v
        self.n_inst += 1
        return v

    def op(self, e, fn, reads=(), writes=(), pe_acc=False):
        track = self.cur[e]
        if self.cnt[track] >= EPOCH:
            self._new_compute_track(e)
            track = self.cur[e]
        deps = self._deps(reads, writes)
        skip = None
        if pe_acc:
            skip = "__pe__"
            deps = {t: v for t, v in deps.items() if not t.startswith("pe")}
        self._wait(e, deps, skip)
        inst = fn(self.eng[e])
        self._commit(e, inst, track, reads, writes)

    def dma(self, q, out, in_, reads=(), writes=(), **kw):
        tl = self.dma_tracks[q]
        track = tl[self.dma_rr[q] % len(tl)]
        self.dma_rr[q] += 1
        if self.cnt[track] >= DMA_EPOCH:
            idx = tl.index(track)
            track = self._new_dma_track(q)
            tl.pop()
            tl[idx] = track
        deps = self._deps(reads, writes)
        if self.cnt[track] > 0:
            deps[track] = max(deps.get(track, 0), self.cnt[track])
        self._wait(q, deps)
        inst = self.eng[q].dma_start(out=out, in_=in_, **kw)
        self._commit(q, inst, track, reads, writes)

    def barrier(self):
        allt = {t: v for t, v in self.cnt.items() if v > 0}
        for e in self.eng:
            self._wait(e, allt)

    def finish(self):
        allt = {t: v for t, v in self.cnt.items() if v > 0}
        self._wait("sp", allt)


DM = 1024
EI = 2048
T_LAT = 2048
T_CTX = 256
T_ALL = T_LAT + T_CTX
NT = T_ALL // 128
DEPTH = 4
EPS = 1e-6
GRID_W = 64
TCH = [(0, 512), (512, 512), (1024, 512), (1536, 512), (2048, 256)]


class Ctx:
    pass


class Scope:
    _uid = [0]

    def __init__(self, C):
        self.C = C
        self.es = ExitStack()

    def __enter__(self):
        self.es.__enter__()
        return self

    def __exit__(self, *a):
        self.C.k.barrier()
        return self.es.__exit__(*a)

    def _name(self, n):
        Scope._uid[0] += 1
        return f"{n}_{Scope._uid[0]}"

    def sb(self, name, shape, dt=F32):
        return self.es.enter_context(self.C.nc.sbuf_tensor(self._name(name), list(shape), dt))

    def ps(self, name, shape, dt=F32):
        return self.es.enter_context(self.C.nc.psum_tensor(self._name(name), list(shape), dt))


def mm(C, out, lhsT, rhs, start, stop, reads, writes):
    C.k.op("pe", lambda e: e.matmul(out, lhsT, rhs, start=start, stop=stop), reads, writes, pe_acc=True)


def act(C, out, in_, func, reads, writes, **kw):
    C.k.op("act", lambda e: e.activation(out=out, in_=in_, func=func, **kw), reads, writes)


def vcol(C, name, n=1, off=0):
    a = C.vec_off[name] + off
    return C.vecs[:, a:a + n]


def adaln(C, L):
    k = C.k
    with Scope(C) as S:
        wst = [S.sb("mwst", [128, 8, 512]) for _ in range(2)]
        bw = [Buf() for _ in range(2)]
        pe_ = S.ps("pe", [128, 512])
        bpe = Buf()
        pg = [S.ps("pg", [128, 1024]) for _ in range(2)]
        bpg = [Buf(), Buf()]
        tmp = S.sb("tmp", [128, 8])
        btmp = Buf()
        dg = [S.sb("dg", [128, 128]) for _ in range(2)]
        bdg = [Buf(), Buf()]
        mw = C.D[f"mod_w{L}"]
        for g in range(6):
            b = g % 2
            k.dma("sp", wst[b][:], mw[:, g * 512:(g + 1) * 512].rearrange("(k p) n -> p k n", p=128), writes=[bw[b]])
            for f4 in range(4):
                f = g * 4 + f4
                for kk in range(8):
                    mm(C, pe_[:, 2 * f:2 * f + 2], wst[b][:, kk, f4 * 128:(f4 + 1) * 128], C.csT[:, kk, :],
                       kk == 0, kk == 7, [bw[b], C.bcsT], [bpe])
        pe3 = pe_[:, 0:48].rearrange("p (f j) -> p f j", j=2)
        mb = vcol(C, "mod_bT", 24, L * 24)
        ng = vcol(C, "norm_gT", 8, L * 8)
        for j in range(2):
            k.op("dve", lambda e: e.tensor_tensor(out=C.eT[:, :, j], in0=pe3[:, :, j], in1=mb, op=ALU.add),
                 [bpe, C.bvecs], [C.beT])
        for j in range(2):
            k.op("dve", lambda e: e.tensor_scalar(tmp[:], C.eT[:, 8:16, j], 1.0, None, op0=ALU.add), [C.beT], [btmp])
            k.op("dve", lambda e: e.tensor_tensor(out=C.gsT[:, :, j], in0=tmp[:], in1=ng, op=ALU.mult),
                 [btmp, C.bvecs], [C.bgsT])
        for j in range(2):
            for kk in range(8):
                b = kk % 2
                k.op("dve", lambda e: e.tensor_scalar(dg[b][:], C.ident_f[:], C.eT[:, 16 + kk, j:j + 1], None, op0=ALU.mult),
                     [C.bconst, C.beT], [bdg[b]])
                mm(C, pg[j][:, kk * 128:(kk + 1) * 128], C.ones_f[:], dg[b][:], True, True, [C.bconst, bdg[b]], [bpg[j]])
            k.op("act", lambda e: e.copy(C.gate_bc[:, j, :], pg[j][:]), [bpg[j]], [C.bgate])


def h_src(C, L, first, i):
    if L == C.layers[0] and first:
        if i < 16:
            return C.D["x"][i * 128:(i + 1) * 128, :]
        return C.D["ctx"][(i - 16) * 128:(i - 15) * 128, :]
    return C.D["hscr"][i * 128:(i + 1) * 128, :]


def norm_phase(C, L, uT, buT):
    k = C.k
    with Scope(C) as S:
        hb = [S.sb("hb", [128, DM]) for _ in range(2)]
        bhb = [Buf(), Buf()]
        junk = S.sb("junk", [128, DM], BF16)
        bjunk = Buf()
        hn = [S.sb("hn", [128, DM], BF16) for _ in range(4)]
        bhn = [Buf() for _ in range(4)]
        st = S.sb("st", [128, 4, 4])
        bst = [Buf() for _ in range(4)]
        tp = [S.ps("tp", [128, 8, 128], BF16) for _ in range(2)]
        btp = [Buf(), Buf()]
        for g in range(5):
            tiles = list(range(4 * g, min(4 * g + 4, NT)))
            j = 0 if g < 4 else 1
            for s, i in enumerate(tiles):
                b = i % 2
                k.dma("sp", hb[b][:], h_src(C, L, True, i), reads=[C.bh[i]], writes=[bhb[b]])
                k.op("pool", lambda e: e.memset(st[:, s, 0:1], 0.0), [], [bst[s]])
                act(C, junk[:], hb[b][:], AF.Square, [bhb[b]], [bjunk, bst[s]], accum_out=st[:, s, 0:1])
                k.op("dve", lambda e: e.tensor_scalar(st[:, s, 1:2], st[:, s, 0:1], 1.0 / DM, EPS, op0=ALU.mult, op1=ALU.add),
                     [bst[s]], [bst[s]])
                act(C, st[:, s, 2:3], st[:, s, 1:2], AF.Sqrt, [bst[s]], [bst[s]])
                k.op("dve", lambda e: e.reciprocal(st[:, s, 3:4], st[:, s, 2:3]), [bst[s]], [bst[s]])
                k.op("dve", lambda e: e.tensor_scalar(hn[s][:], hb[b][:], st[:, s, 3:4], None, op0=ALU.mult),
                     [bhb[b], bst[s]], [bhn[s]])
            n = len(tiles)
            for kk in range(8):
                b = kk % 2
                for s, i in enumerate(tiles):
                    C.k.op("pe", lambda e: e.transpose(tp[b][:, s, :], hn[s][:, kk * 128:(kk + 1) * 128], C.ident_b[:]),
                           [bhn[s], C.bconst], [btp[b]], pe_acc=True)
                t0 = tiles[0] * 128
                act(C, uT[:, kk, t0:t0 + n * 128], tp[b][:, 0:n, :].rearrange("p s t -> p (s t)"), AF.Identity,
                    [btp[b], C.bgsT, C.beT], [buT], scale=C.gsT[:, kk, j:j + 1], bias=C.eT[:, kk, j:j + 1])


def outproj_pass(C, L, S, yT4, byT, w_out, e0, first, last, bank, bbank):
    k = C.k
    need_ctx = L < DEPTH - 1
    R = C.op_bufs
    for c in range(4):
        b = c % 2
        k.dma("sp", R.wst[b][:], w_out[(e0 + c) * 128:(e0 + c + 1) * 128, :], writes=[R.bwst[b]])
        k.op("pool", lambda e: e.tensor_copy(R.wo[:, c, :], R.wst[b][:]), [R.bwst[b]], [R.bwo])
    ntiles = NT if need_ctx else 16
    for i in range(ntiles):
        j = 0 if i < 16 else 1
        b = i % 2
        k.dma("sp", R.hb[b][:], h_src(C, L, first, i), reads=[C.bh[i]], writes=[R.bhb[b]])
        for n in range(2):
            pb = (2 * i + n) % 2
            for c in range(4):
                mm(C, bank[pb][:, :], yT4[:, c, i * 128:(i + 1) * 128], R.wo[:, c, n * 512:(n + 1) * 512],
                   c == 0, c == 3, [byT, R.bwo], [bbank[pb]])
            k.op("dve", lambda e: e.tensor_tensor(out=R.tmp[pb][:], in0=bank[pb][:, :], in1=C.gate_bc[:, j, n * 512:(n + 1) * 512], op=ALU.mult),
                 [bbank[pb], C.bgate], [R.btmp[pb]])
            k.op("pool", lambda e: e.tensor_tensor(out=R.hb[b][:, n * 512:(n + 1) * 512], in0=R.hb[b][:, n * 512:(n + 1) * 512],
                                                   in1=R.tmp[pb][:], op=ALU.add),
                 [R.btmp[pb], R.bhb[b]], [R.bhb[b]])
        if last and L == DEPTH - 1:
            dst = C.D["out"][i * 128:(i + 1) * 128, :]
            k.dma("pool", dst, R.hb[b][:], reads=[R.bhb[b]], writes=[C.bout])
        else:
            dst = C.D["hscr"][i * 128:(i + 1) * 128, :]
            k.dma("pool", dst, R.hb[b][:], reads=[R.bhb[b]], writes=[C.bh[i]])


def alloc_outproj_bufs(C, S):
    R = Ctx()
    R.wst = [S.sb("owst", [128, DM]) for _ in range(2)]
    R.bwst = [Buf(), Buf()]
    R.wo = S.sb("wo", [128, 4, DM], BF16)
    R.bwo = Buf()
    R.hb = [S.sb("ohb", [128, DM]) for _ in range(2)]
    R.bhb = [Buf(), Buf()]
    R.tmp = [S.sb("otmp", [128, 512]) for _ in range(2)]
    R.btmp = [Buf(), Buf()]
    C.op_bufs = R


def load_w_slices(C, W, w_in, cols):
    k = C.k
    for n, c0 in enumerate(cols):
        b = n % 2
        k.dma("sp", W.wst[b][:], w_in[:, c0:c0 + 128].rearrange("(k p) n -> p k n", p=128), writes=[W.bwst[b]])
        k.op("pool", lambda e: e.tensor_copy(W.w[n][:], W.wst[b][:]), [W.bwst[b]], [W.bw[n]])


def proj_qk(C, W, P, wi, gcol, dst, bdst, chunks, rope):
    k = C.k
    for ci, (c0, n) in enumerate(chunks):
        pj, bpj = P.bank[ci % 2], P.bbank[ci % 2]
        for kk in range(8):
            mm(C, pj[:, :n], W.w[wi][:, kk, :], P.uT[:, kk, c0:c0 + n], kk == 0, kk == 7, [W.bw[wi], P.buT], [bpj])
        act(C, P.sq[:, :n], pj[:, :n], AF.Square, [bpj], [P.bsq])
        mm(C, P.bank[2][:, :n], C.onesblk_b[:], P.sq[:, :n], True, True, [P.bsq, C.bconst], [P.bbank[2]])
        act(C, P.rs[:, :n], P.bank[2][:, :n], AF.Sqrt, [P.bbank[2], C.bconst], [P.brs], bias=C.eps_col[:], scale=1.0)
        k.op("dve", lambda e: e.reciprocal(P.rs[:, :n], P.rs[:, :n]), [P.brs], [P.brs])
        is_lat = c0 < T_LAT
        if rope and is_lat:
            k.op("dve", lambda e: e.scalar_tensor_tensor(out=P.kn[:, :n], in0=pj[:, :n], scalar=gcol, in1=P.rs[:, :n],
                                                         op0=ALU.mult, op1=ALU.mult), [bpj, P.brs, C.bvecs, P.bg], [P.bkn])
            act(C, P.knb[:, :n], P.kn[:, :n], AF.Copy, [P.bkn], [P.bknb])
            mm(C, P.bank[3][:, :n], C.rot_b[:], P.knb[:, :n], True, True, [P.bknb, C.bconst], [P.bbank[3]])
            k.op("dve", lambda e: e.tensor_tensor(out=P.t1[:, :n], in0=P.kn[:, :n], in1=C.cosT[:, c0:c0 + n], op=ALU.mult),
                 [P.bkn, C.bconst], [P.bt1])
            k.op("dve", lambda e: e.tensor_tensor(out=P.t2[:, :n], in0=P.bank[3][:, :n], in1=C.sinT[:, c0:c0 + n], op=ALU.mult),
                 [P.bbank[3], C.bconst], [P.bt2])
            k.op("pool", lambda e: e.tensor_tensor(out=dst[:, c0:c0 + n], in0=P.t1[:, :n], in1=P.t2[:, :n], op=ALU.add),
                 [P.bt1, P.bt2], [bdst])
        else:
            k.op("dve", lambda e: e.scalar_tensor_tensor(out=dst[:, c0:c0 + n], in0=pj[:, :n], scalar=gcol, in1=P.rs[:, :n],
                                                         op0=ALU.mult, op1=ALU.mult), [bpj, P.brs, C.bvecs, P.bg], [bdst])


def proj_z(C, W, P, wi, dst, bdst, chunks):
    for ci, (c0, n) in enumerate(chunks):
        pj, bpj = P.bank[ci % 2], P.bbank[ci % 2]
        for kk in range(8):
            mm(C, pj[:, :n], W.w[wi][:, kk, :], P.uT[:, kk, c0:c0 + n], kk == 0, kk == 7, [W.bw[wi], P.buT], [bpj])
        act(C, dst[:, c0:c0 + n], pj[:, :n], AF.Silu, [bpj], [bdst])


def alloc_attn_common(C, S, uT, buT):
    W = Ctx()
    W.wst = [S.sb("wst", [128, 8, 128]) for _ in range(2)]
    W.bwst = [Buf(), Buf()]
    W.w = [S.sb("wbf", [128, 8, 128], BF16) for _ in range(4)]
    W.bw = [Buf() for _ in range(4)]
    P = Ctx()
    P.uT, P.buT = uT, buT
    P.bank = [S.ps("bank", [128, 512]) for _ in range(8)]
    P.bbank = [Buf() for _ in range(8)]
    P.sq = S.sb("sq", [128, 512], BF16); P.bsq = Buf()
    P.rs = S.sb("rs", [128, 512]); P.brs = Buf()
    P.kn = S.sb("kn", [128, 512]); P.bkn = Buf()
    P.knb = S.sb("knb", [128, 512], BF16); P.bknb = Buf()
    P.t1 = S.sb("t1", [128, 512]); P.bt1 = Buf()
    P.t2 = S.sb("t2", [128, 512]); P.bt2 = Buf()
    P.g = S.sb("gcols", [128, 8]); P.bg = Buf()
    P.qT = S.sb("qT", [128, T_ALL], BF16); P.bqT = Buf()
    P.kT = S.sb("kT", [128, T_ALL], BF16); P.bkT = Buf()
    P.szT = S.sb("szT", [128, T_ALL], BF16); P.bszT = Buf()
    P.pT = [S.sb("pT", [128, 1024], BF16) for _ in range(3)]
    P.bpT = [Buf() for _ in range(3)]
    return W, P


def diff_layer(C, L):
    k = C.k
    j = L // 3
    need_ctx = L < DEPTH - 1
    lam_init = 0.8 - 0.6 * math.exp(-0.3 * L)
    w_in = C.D[f"a_w_in{j}"]
    w_out = C.D[f"a_w_out{j}"]
    adaln(C, L)
    with Scope(C) as S:
        uT = S.sb("uT", [128, 8, T_ALL], BF16)
        buT = Buf()
        norm_phase(C, L, uT, buT)
        C.cosT = S.sb("cos_sb", [128, T_LAT]); C.sinT = S.sb("sin_sb", [128, T_LAT]); C.brope = Buf()
        k.dma("sp", C.cosT[:], C.D["cosT"], writes=[C.brope])
        k.dma("sp", C.sinT[:], C.D["sinT"], writes=[C.brope])
        k.barrier()
        W, P = alloc_attn_common(C, S, uT, buT)
        alloc_outproj_bufs(C, S)
        yT4 = S.sb("yT4", [128, 4, T_ALL], BF16); byT = Buf()
        v = S.sb("v", [128, NT, 128], BF16); bv = Buf()
        r1 = S.sb("r1", [128, 512]); br1 = Buf()
        r2 = S.sb("r2", [128, 512]); br2 = Buf()
        o = S.sb("o", [128, 512]); bo = Buf()
        lamt = S.sb("lamt", [1, 8, 64]); blam = Buf()
        k.op("dve", lambda e: e.tensor_scalar(P.g[:, 0:1], vcol(C, "a_qn", 1, j), 0.125, None, op0=ALU.mult), [C.bvecs], [P.bg])
        k.op("dve", lambda e: e.tensor_copy(P.g[:, 1:2], vcol(C, "a_kn", 1, j)), [C.bvecs], [P.bg])
        k.op("dve", lambda e: e.tensor_scalar(P.g[:, 2:3], vcol(C, "a_sub", 1, j), 1.0 - lam_init, None, op0=ALU.mult), [C.bvecs], [P.bg])
        k.dma("sp", lamt[:, 0:4, :], C.D["a_lam"][j:j + 1, :, :], writes=[blam])
        lv = lamt[:, 0:4, :].rearrange("p (a b) d -> p a b d", b=2)
        k.op("dve", lambda e: e.tensor_tensor(out=lamt[:, 4:6, :], in0=lv[:, :, 0, :], in1=lv[:, :, 1, :], op=ALU.mult), [blam], [blam])
        k.op("dve", lambda e: e.tensor_reduce(out=lamt[:, 6, 0:2], in_=lamt[:, 4:6, :], axis=AX.X, op=ALU.add), [blam], [blam])
        act(C, lamt[:, 6, 2:4], lamt[:, 6, 0:2], AF.Exp, [blam], [blam])
        k.op("dve", lambda e: e.tensor_tensor(out=lamt[:, 6, 4:5], in0=lamt[:, 6, 3:4], in1=lamt[:, 6, 2:3], op=ALU.subtract), [blam], [blam])
        k.op("dve", lambda e: e.tensor_scalar(lamt[:, 6, 5:6], lamt[:, 6, 4:5], -lam_init, None, op0=ALU.add), [blam], [blam])
        mm(C, P.bank[2][:, 0:1], C.ones_f[0:1, :], lamt[:, 6, 5:6], True, True, [blam, C.bconst], [P.bbank[2]])
        k.op("dve", lambda e: e.tensor_copy(P.g[:, 3:4], P.bank[2][:, 0:1]), [P.bbank[2]], [P.bg])

        q_chunks = TCH if need_ctx else TCH[:4]
        for hd in range(16):
            load_w_slices(C, W, w_in, [hd * 128, 2048 + hd * 128, 4096 + hd * 128, 6144 + hd * 128])
            proj_qk(C, W, P, 1, P.g[:, 1:2], P.kT, P.bkT, TCH, True)
            proj_qk(C, W, P, 0, P.g[:, 0:1], P.qT, P.bqT, q_chunks, True)
            proj_z(C, W, P, 3, P.szT, P.bszT, q_chunks)
            for g in range(5):
                tiles = list(range(4 * g, min(4 * g + 4, NT)))
                pj, bpj = P.bank[g % 2], P.bbank[g % 2]
                for s, i in enumerate(tiles):
                    for kk in range(8):
                        mm(C, pj[:, s * 128:(s + 1) * 128], uT[:, kk, i * 128:(i + 1) * 128], W.w[2][:, kk, :],
                           kk == 0, kk == 7, [buT, W.bw[2]], [bpj])
                n = len(tiles)
                act(C, v[:, 4 * g:4 * g + n, :].rearrange("p s e -> p (s e)"), pj[:, :n * 128], AF.Copy, [bpj], [bv])
            qsets = [(qc * 512, 512, list(range(NT))) for qc in range(4)]
            if need_ctx:
                qsets.append((T_LAT, T_CTX, [16, 17]))
            for (q0, n, kts) in qsets:
                for sub in range(2):
                    psl = slice(sub * 64, (sub + 1) * 64)
                    Ob, bOb = P.bank[4 + sub], P.bbank[4 + sub]
                    Sb, bSb = P.bank[6 + sub], P.bbank[6 + sub]

                    def smm(ix):
                        kt = kts[ix]
                        mm(C, P.bank[ix % 2][:, :n], P.kT[psl, kt * 128:(kt + 1) * 128], P.qT[psl, q0:q0 + n], True, True,
                           [P.bkT, P.bqT], [P.bbank[ix % 2]])
                    smm(0)
                    for ix, kt in enumerate(kts):
                        pb = ix % 3
                        act(C, P.pT[pb][:, :n], P.bank[ix % 2][:, :n], AF.Exp, [P.bbank[ix % 2]], [P.bpT[pb]])
                        if ix + 1 < len(kts):
                            smm(ix + 1)
                        mm(C, Ob[:, :n], v[:, kt, :], P.pT[pb][:, :n], ix == 0, ix == len(kts) - 1, [bv, P.bpT[pb]], [bOb])
                        mm(C, Sb[:, :n], C.ones_b[:], P.pT[pb][:, :n], ix == 0, ix == len(kts) - 1, [C.bconst, P.bpT[pb]], [bSb])
                k.op("dve", lambda e: e.reciprocal(r1[:, :n], P.bank[6][:, :n]), [P.bbank[6]], [br1])
                k.op("dve", lambda e: e.reciprocal(r2[:, :n], P.bank[7][:, :n]), [P.bbank[7]], [br2])
                k.op("dve", lambda e: e.tensor_tensor(out=r1[:, :n], in0=P.bank[4][:, :n], in1=r1[:, :n], op=ALU.mult), [P.bbank[4], br1], [br1])
                k.op("dve", lambda e: e.tensor_tensor(out=r2[:, :n], in0=P.bank[5][:, :n], in1=r2[:, :n], op=ALU.mult), [P.bbank[5], br2], [br2])
                k.op("dve", lambda e: e.scalar_tensor_tensor(out=o[:, :n], in0=r2[:, :n], scalar=P.g[:, 3:4], in1=r1[:, :n],
                                                             op0=ALU.mult, op1=ALU.add), [br1, br2, P.bg], [bo])
                act(C, P.sq[:, :n], o[:, :n], AF.Square, [bo], [P.bsq])
                mm(C, P.bank[2][:, :n], C.ones128_b[:], P.sq[:, :n], True, True, [P.bsq, C.bconst], [P.bbank[2]])
                act(C, P.rs[:, :n], P.bank[2][:, :n], AF.Sqrt, [P.bbank[2], C.bconst], [P.brs], bias=C.eps_col[:], scale=1.0)
                k.op("dve", lambda e: e.reciprocal(P.rs[:, :n], P.rs[:, :n]), [P.brs], [P.brs])
                k.op("dve", lambda e: e.tensor_tensor(out=o[:, :n], in0=o[:, :n], in1=P.rs[:, :n], op=ALU.mult), [bo, P.brs], [bo])
                k.op("dve", lambda e: e.scalar_tensor_tensor(out=yT4[:, hd % 4, q0:q0 + n], in0=o[:, :n], scalar=P.g[:, 2:3],
                                                             in1=P.szT[:, q0:q0 + n], op0=ALU.mult, op1=ALU.mult),
                     [bo, P.bg, P.bszT], [byT])
            if hd % 4 == 3:
                outproj_pass(C, L, S, yT4, byT, w_out, (hd // 4) * 4, hd == 3, hd == 15, P.bank, P.bbank)


def _colT(v, n):
    return np.ascontiguousarray(np.asarray(v, np.float32).reshape(n, 128).T)


def make_vec_layout():
    names = [("cT", 8), ("cctxT", 8), ("mod_bT", 96), ("norm_gT", 32), ("a_qn", 2), ("a_kn", 2), ("a_sub", 2),
             ("b_qn", 1), ("b_kn", 1), ("c_convw", 64), ("c_convb", 16), ("c_mhn", 16), ("c_skip", 16)]
    off = {}
    o = 0
    for n, w in names:
        off[n] = o
        o += w
    return off, o


def const_mats():
    m = np.zeros((128, 6, 128), np.float32)
    m[:, 0, :] = np.eye(128)
    m[:, 1, :] = 1.0
    for h in range(2):
        m[h * 64:(h + 1) * 64, 2, h * 64:(h + 1) * 64] = 1.0 / 64
    m[:, 3, :] = 1.0 / 128
    for h in range(2):
        for a in range(2):
            for f in range(16):
                d0 = h * 64 + a * 32 + f
                d1 = d0 + 16
                m[d1, 4, d0] = -1.0
                m[d0, 4, d1] = 1.0
    m[:, 5, :] = np.eye(128)[::-1]
    return m


def rope_tables():
    t = np.arange(T_LAT)
    row = (t // GRID_W).astype(np.float32)
    col = (t % GRID_W).astype(np.float32)
    inv = (np.float32(10000.0) ** (-np.arange(16, dtype=np.float32) / np.float32(16))).astype(np.float32)
    ar = row[:, None] * inv
    ac = col[:, None] * inv
    ang = np.concatenate([ar, ar, ac, ac], axis=-1).astype(np.float32)
    cos = np.cos(ang).astype(np.float32).T
    sin = np.sin(ang).astype(np.float32).T
    return (np.ascontiguousarray(np.concatenate([cos, cos], 0)), np.ascontiguousarray(np.concatenate([sin, sin], 0)))


def prep_inputs(inp):
    off, nv = make_vec_layout()
    B = inp["x"].shape[0]
    shared = {}
    for L in range(DEPTH):
        shared[f"mod_w{L}"] = np.ascontiguousarray(inp["mod_w"][L], dtype=np.float32)
    for j in range(2):
        shared[f"a_w_in{j}"] = np.ascontiguousarray(inp["a_w_in"][j], dtype=np.float32)
        shared[f"a_w_out{j}"] = np.ascontiguousarray(inp["a_w_out"][j], dtype=np.float32)
    shared["a_lam"] = np.ascontiguousarray(inp["a_lam"], dtype=np.float32)
    for name in ("b_w_in", "b_w_out", "c_w_in", "c_w_out"):
        shared[name] = np.ascontiguousarray(inp[name][0], dtype=np.float32)
    shared.update(mlstm_host_layout(inp))
    shared["cmats"] = const_mats()
    shared["cosT"], shared["sinT"] = rope_tables()
    idx, valid = na_table_index()
    rpb = np.asarray(inp["b_rpb"][0], np.float32).reshape(32, -1)
    rpb_pad = np.concatenate([rpb, np.zeros((32, 1), np.float32)], axis=1)
    shared["nab"] = np.ascontiguousarray(rpb_pad[:, idx.reshape(-1)].reshape(32, 128, NA_NT * 128))
    shared["namask"] = np.ascontiguousarray(np.where(valid, 0.0, -30000.0).astype(np.float32).reshape(128, NA_NT * 128))
    per = []
    for b in range(B):
        v = np.zeros((128, nv), np.float32)
        v[:, off["cT"]:off["cT"] + 8] = _colT(inp["c"][b], 8)
        v[:, off["cctxT"]:off["cctxT"] + 8] = _colT(inp["c_ctx"], 8)
        for L in range(DEPTH):
            v[:, off["mod_bT"] + L * 24:off["mod_bT"] + (L + 1) * 24] = _colT(inp["mod_b"][L], 24)
            v[:, off["norm_gT"] + L * 8:off["norm_gT"] + (L + 1) * 8] = _colT(inp["norm_g"][L], 8)
        for j in range(2):
            v[:, off["a_qn"] + j] = np.tile(inp["a_qn_g"][j], 2)
            v[:, off["a_kn"] + j] = np.tile(inp["a_kn_g"][j], 2)
            v[:, off["a_sub"] + j] = inp["a_subln_g"][j]
        v[:, off["b_qn"]] = np.tile(inp["b_qn_g"][0], 2)
        v[:, off["b_kn"]] = np.tile(inp["b_kn_g"][0], 2)
        v[:, off["c_convw"]:off["c_convw"] + 64] = np.ascontiguousarray(
            inp["c_conv_w"][0].T.reshape(16, 128, 4).transpose(1, 0, 2).reshape(128, 64))
        v[:, off["c_convb"]:off["c_convb"] + 16] = _colT(inp["c_conv_b"][0], 16)
        v[:, off["c_mhn"]:off["c_mhn"] + 16] = _colT(inp["c_mhn_g"][0], 16)
        v[:, off["c_skip"]:off["c_skip"] + 16] = _colT(inp["c_skip"][0], 16)
        d = dict(shared)
        d["vecs"] = v
        d["x"] = np.ascontiguousarray(inp["x"][b], dtype=np.float32)
        d["ctx"] = np.ascontiguousarray(inp["ctx"][b], dtype=np.float32)
        per.append(d)
    return per


LAYER_FN = {}


def declare_mlstm_inputs(C, din):
    din("cBD", [3, 16, 128, 128]); din("cBDT", [3, 16, 128, 128]); din("cG", [3, 16, 128, 16])
    din("cgb", [64, 2]); din("csel", [64, 8, 128]); din("cmask", [128, 2, 64])


def mlstm_host_layout(inp):
    out = {}
    bd = np.zeros((3, 16, 128, 128), np.float32)
    for a, name in enumerate(("c_wq", "c_wk", "c_wv")):
        w = np.asarray(inp[name][0], np.float32)
        for fc in range(16):
            for n in range(32):
                bd[a, fc, 4 * n:4 * n + 4, 4 * n:4 * n + 4] = w[fc * 32 + n]
    out["cBD"] = bd
    out["cBDT"] = np.ascontiguousarray(bd.transpose(0, 1, 3, 2))
    gw = np.asarray(inp["c_gate_w"][0], np.float32)
    g = gw.transpose(1, 0, 2).reshape(6144, 16)
    out["cG"] = np.ascontiguousarray(g.reshape(3, 16, 128, 16))
    gb = np.asarray(inp["c_gate_b"][0], np.float32)
    cgb = np.zeros((64, 2), np.float32)
    for d in range(2):
        for h in range(4):
            cgb[d * 32 + h, 0] = gb[d, h]
            cgb[d * 32 + h, 1] = gb[d, 4 + h]
    out["cgb"] = cgb
    sel = np.zeros((64, 8, 128), np.float32)
    for d in range(2):
        for h in range(4):
            sel[d * 32 + h, d * 4 + h, :] = 1.0
    out["csel"] = sel
    mk = np.zeros((128, 2, 64), np.float32)
    s_ = (np.arange(128) % 64)[:, None]
    t_ = np.arange(64)[None, :]
    mk[:, 0, :] = (s_ <= t_)
    mk[:, 1, :] = (s_ >= t_)
    out["cmask"] = mk
    return out


def build_program(layers=(0, 1, 2, 3), debug=False, S=1):
    nc = bass.Bass("TRN2", target_bir_lowering=False)
    C = Ctx()
    C.nc = nc
    C.layers = list(layers)
    C.D = {}
    off, nv = make_vec_layout()
    C.vec_off = off

    C.in_names = []

    def din(name, shape):
        C.D[name] = nc.dram_tensor(name, list(shape), F32, kind="ExternalInput").ap()
        C.in_names.append(name)

    C.S = S
    for si in range(S):
        din(f"x{si}", [T_LAT, DM]); din(f"ctx{si}", [T_CTX, DM]); din(f"vecs{si}", [128, nv])
    din("cmats", [128, 6, 128])
    din("cosT", [128, T_LAT]); din("sinT", [128, T_LAT])
    for L in C.layers:
        din(f"mod_w{L}", [DM, 3 * DM])
    if 0 in C.layers:
        din("a_w_in0", [DM, 4 * EI]); din("a_w_out0", [EI, DM])
    if 3 in C.layers:
        din("a_w_in1", [DM, 4 * EI]); din("a_w_out1", [EI, DM])
    if 0 in C.layers or 3 in C.layers:
        din("a_lam", [2, 4, 64])
    if 1 in C.layers:
        din("b_w_in", [DM, 4 * EI]); din("b_w_out", [EI, DM])
        din("nab", [32, 128, NA_NT * 128]); din("namask", [128, NA_NT * 128])
    if 2 in C.layers:
        din("c_w_in", [DM, 3 * EI]); din("c_w_out", [EI, DM])
        declare_mlstm_inputs(C, din)
    for si in range(S):
        C.D[f"out{si}"] = nc.dram_tensor(f"out{si}", [T_LAT, DM], F32, kind="ExternalOutput").ap()
    C.D["hscr"] = nc.dram_tensor("hscr", [T_ALL, DM], F32, kind="ExternalOutput" if debug else "Internal").ap()
    C.bh = [Buf() for _ in range(NT)]
    C.bout = Buf()
    es = ExitStack()
    with es:
        C.k = K(nc, es)
        k = C.k

        def sb(name, shape, dt=F32):
            return es.enter_context(nc.sbuf_tensor(name, list(shape), dt))
        C.vecs = sb("vecs_sb", [128, nv]); C.bvecs = Buf()
        cm = sb("cmats_sb", [128, 6, 128]); C.bconst = Buf()
        cb = sb("cmats_bf", [128, 6, 128], BF16)
        C.eps_col = sb("eps_col", [128, 1])
        C.csT = sb("csT", [128, 8, 2]); C.bcsT = Buf()
        C.eT = sb("eT", [128, 24, 2]); C.beT = Buf()
        C.gsT = sb("gsT", [128, 8, 2]); C.bgsT = Buf()
        C.gate_bc = sb("gate_bc", [128, 2, DM]); C.bgate = Buf()
        k.dma("sp", cm[:], C.D["cmats"], writes=[C.bconst])
        k.barrier()
        k.op("dve", lambda e: e.tensor_copy(cb[:], cm[:]), [C.bconst], [C.bconst])
        k.op("pool", lambda e: e.memset(C.eps_col[:], EPS), [], [C.bconst])
        k.barrier()
        C.ident_f = cm[:, 0, :]; C.ones_f = cm[:, 1, :]
        C.ident_b = cb[:, 0, :]; C.ones_b = cb[:, 1, :]; C.onesblk_b = cb[:, 2, :]; C.ones128_b = cb[:, 3, :]
        C.rot_b = cb[:, 4, :]
        for si in range(S):
            C.D["x"] = C.D[f"x{si}"]; C.D["ctx"] = C.D[f"ctx{si}"]; C.D["out"] = C.D[f"out{si}"]
            k.barrier()
            k.dma("sp", C.vecs[:], C.D[f"vecs{si}"], writes=[C.bvecs])
            act(C, C.csT[:, :, 0], vcol(C, "cT", 8), AF.Silu, [C.bvecs], [C.bcsT])
            act(C, C.csT[:, :, 1], vcol(C, "cctxT", 8), AF.Silu, [C.bvecs], [C.bcsT])
            k.barrier()
            for L in C.layers:
                LAYER_FN[L % 3](C, L)
        if debug and (DEPTH - 1) not in C.layers:
            pass
        k.finish()
    C.n_inst = k.n_inst
    return nc, C


LAYER_FN[0] = diff_layer


NA_NT = 12


def na_table_index():
    idx = np.full((128, NA_NT, 128), 465, np.int64)
    valid = np.zeros((128, NA_NT, 128), bool)
    kc = np.arange(64)[:, None]
    c = np.arange(64)[None, :]
    ws = np.clip(c - 8, 0, 48)
    col_ok = (kc >= ws) & (kc < ws + 16)
    dc = np.clip(kc - c + 15, 0, 30)
    for t in range(NA_NT):
        interior = t < 5
        o = t - 2 if interior else t - 8
        for p in range(2):
            for par in range(2):
                dr = 2 * o + p - par
                row_ok = (-4 <= dr <= 3) if interior else (-7 <= dr <= 7)
                if not row_ok:
                    continue
                blk_valid = col_ok
                blk_idx = np.where(blk_valid, (dr + 7) * 31 + dc, 465)
                idx[p * 64:(p + 1) * 64, t, par * 64:(par + 1) * 64] = blk_idx
                valid[p * 64:(p + 1) * 64, t, par * 64:(par + 1) * 64] = blk_valid
    return idx, valid


def na_keys(m):
    if m >= 16:
        return [(16, None), (17, None)]
    if 2 <= m <= 13:
        ks = [(m + o, o + 2) for o in range(-2, 3)]
    elif m < 2:
        ks = [(jj, 5 + (jj - m) + 3) for jj in range(0, 4)]
    else:
        ks = [(jj, 5 + (jj - m) + 3) for jj in range(12, 16)]
    return ks + [(16, None), (17, None)]


def na_layer(C, L):
    k = C.k
    w_in = C.D["b_w_in"]
    w_out = C.D["b_w_out"]
    adaln(C, L)
    with Scope(C) as S:
        uT = S.sb("uT", [128, 8, T_ALL], BF16)
        buT = Buf()
        norm_phase(C, L, uT, buT)
        W, P = alloc_attn_common(C, S, uT, buT)
        alloc_outproj_bufs(C, S)
        yT4 = S.sb("yT4", [128, 4, T_ALL], BF16); byT = Buf()
        vaug = S.sb("vaug", [128, NT, 256], BF16); bva = Buf()
        tst = [S.sb("tst", [128, NA_NT * 128]) for _ in range(2)]; btst = [Buf(), Buf()]
        tab = [S.sb("tab", [128, NA_NT, 128], BF16) for _ in range(2)]; btab = [Buf(), Buf()]
        mask = S.sb("namask", [128, NA_NT * 128]); bmask = Buf()
        rrow = S.sb("rrow", [128, 128]); brrow = Buf()
        tz = S.sb("tz", [128, 128]); btz = Buf()
        k.dma("sp", mask[:], C.D["namask"], writes=[bmask])
        k.op("dve", lambda e: e.tensor_scalar(P.g[:, 0:1], vcol(C, "b_qn", 1), 0.125, None, op0=ALU.mult), [C.bvecs], [P.bg])
        k.op("dve", lambda e: e.tensor_copy(P.g[:, 1:2], vcol(C, "b_kn", 1)), [C.bvecs], [P.bg])
        k.op("dve", lambda e: e.memset(vaug[:, :, 64:192], 0.0), [], [bva])
        k.op("dve", lambda e: e.memset(vaug[:, :, 64:66], 1.0), [bva], [bva])
        k.op("dve", lambda e: e.memset(vaug[:, :, 128:130], 1.0), [bva], [bva])
        for hp in range(int(os.environ.get("NA_HP", "16"))):
            load_w_slices(C, W, w_in, [hp * 128, 2048 + hp * 128, 4096 + hp * 128, 6144 + hp * 128])
            proj_qk(C, W, P, 1, P.g[:, 1:2], P.kT, P.bkT, TCH, False)
            proj_qk(C, W, P, 0, P.g[:, 0:1], P.qT, P.bqT, TCH, False)
            proj_z(C, W, P, 3, P.szT, P.bszT, TCH)
            for g in range(5):
                tiles = list(range(4 * g, min(4 * g + 4, NT)))
                pj, bpj = P.bank[g % 2], P.bbank[g % 2]
                for s, i in enumerate(tiles):
                    for kk in range(8):
                        mm(C, pj[:, s * 128:(s + 1) * 128], uT[:, kk, i * 128:(i + 1) * 128], W.w[2][:, kk, :],
                           kk == 0, kk == 7, [buT, W.bw[2]], [bpj])
                n = len(tiles)
                pv = pj[:, :n * 128].rearrange("p (s e) -> p s e", e=128)
                act(C, vaug[:, 4 * g:4 * g + n, 0:64], pv[:, :, 0:64], AF.Copy, [bpj], [bva])
                act(C, vaug[:, 4 * g:4 * g + n, 192:256], pv[:, :, 64:128], AF.Copy, [bpj], [bva])
            for hh in range(2):
                head = 2 * hp + hh
                k.dma("sp", tst[hh][:], C.D["nab"][head], writes=[btst[hh]])
                k.op("dve", lambda e: e.tensor_tensor(out=tab[hh][:].rearrange("p t q -> p (t q)"), in0=tst[hh][:], in1=mask[:], op=ALU.add),
                     [btst[hh], bmask], [btab[hh]])
            it = 0
            for hh in range(2):
                hsl = slice(hh * 64, (hh + 1) * 64)
                for m in ([int(a) for a in os.environ["NA_MLIST"].split(",")] if "NA_MLIST" in os.environ else range(NT)):
                    keys = na_keys(m)
                    if os.environ.get("NA_STAGE", "9") == "0":
                        continue
                    ns = len(keys)
                    pr = (it % 2) * 2
                    Ob, bOb = P.bank[4 + it % 2], P.bbank[4 + it % 2]
                    Bb, bBb = P.bank[6 + it % 2], P.bbank[6 + it % 2]
                    pT, bpT = P.pT[it % 3], P.bpT[it % 3]
                    it += 1
                    q0 = m * 128
                    for slot, (kt, tbl) in enumerate(keys):
                        bk = pr + slot // 4
                        col = (slot % 4) * 128
                        mm(C, P.bank[bk][:, col:col + 128], P.kT[hsl, kt * 128:(kt + 1) * 128], P.qT[hsl, q0:q0 + 128],
                           True, tbl is None, [P.bkT, P.bqT], [P.bbank[bk]])
                        if tbl is not None:
                            mm(C, P.bank[bk][:, col:col + 128], C.ident_b[:], tab[hh][:, tbl, :], False, True,
                               [btab[hh], C.bconst], [P.bbank[bk]])
                    n0 = min(ns, 4) * 128
                    act(C, pT[:, 0:n0], P.bank[pr][:, 0:n0], AF.Exp, [P.bbank[pr]], [bpT])
                    if ns > 4:
                        n1 = (ns - 4) * 128
                        act(C, pT[:, 512:512 + n1], P.bank[pr + 1][:, 0:n1], AF.Exp, [P.bbank[pr + 1]], [bpT])
                    for slot, (kt, tbl) in enumerate(keys):
                        if hh == 0:
                            mm(C, Ob[:, 0:128], vaug[:, kt, 0:128], pT[:, slot * 128:(slot + 1) * 128], slot == 0, slot == ns - 1,
                               [bva, bpT], [bOb])
                        else:
                            mm(C, Ob[:, 0:128], vaug[:, kt, 128:256], pT[:, slot * 128:(slot + 1) * 128], slot == 0, slot == ns - 1,
                               [bva, bpT], [bOb])
                    if hh == 0:
                        k.op("dve", lambda e: e.reciprocal(rrow[64:65, :], Ob[64:65, 0:128]), [bOb], [brrow])
                        mm(C, Bb[0:64, 0:128], C.ones_f[64:65, 0:64], rrow[64:65, :], True, True, [brrow, C.bconst], [bBb])
                    else:
                        k.op("dve", lambda e: e.reciprocal(rrow[0:1, :], Ob[0:1, 0:128]), [bOb], [brrow])
                        mm(C, Bb[:, 0:128], C.ones_f[0:1, :], rrow[0:1, :], True, True, [brrow, C.bconst], [bBb])
                    k.op("dve", lambda e: e.tensor_tensor(out=tz[hsl, :], in0=Bb[hsl, 0:128], in1=P.szT[hsl, q0:q0 + 128], op=ALU.mult),
                         [bBb, P.bszT], [btz])
                    k.op("dve", lambda e: e.tensor_tensor(out=yT4[hsl, hp % 4, q0:q0 + 128], in0=Ob[hsl, 0:128], in1=tz[hsl, :], op=ALU.mult),
                         [bOb, btz], [byT])
            if hp % 4 == 3:
                outproj_pass(C, L, S, yT4, byT, w_out, (hp // 4) * 4, hp == 3, hp == 15, P.bank, P.bbank)


LAYER_FN[1] = na_layer


def mlstm_layer(C, L):
    k = C.k
    nc = C.nc
    w_in = C.D["c_w_in"]
    w_out = C.D["c_w_out"]
    if "xmT_d" not in C.D:
        C.D["xmT_d"] = nc.dram_tensor("xmT_d", [16, 128, T_ALL], BF16).ap()
        C.D["xcT_d"] = nc.dram_tensor("xcT_d", [16, 128, T_ALL], BF16).ap()
        C.D["szT_d"] = nc.dram_tensor("szT_d", [16, 128, T_ALL], F32).ap()
        C.D["sog_d"] = nc.dram_tensor("sog_d", [T_ALL, EI], F32).ap()
    xmT_d, xcT_d, szT_d, sog_d = C.D["xmT_d"], C.D["xcT_d"], C.D["szT_d"], C.D["sog_d"]
    bxm = [Buf() for _ in range(16)]; bxc = [Buf() for _ in range(16)]; bsz = [Buf() for _ in range(16)]
    bsog = Buf()
    adaln(C, L)
    with Scope(C) as S0:
        QT = S0.sb("QT", [128, NT, 4, 36]); bQT = Buf()
        NAr = S0.sb("NAr", [64, T_ALL]); bNAr = Buf()
        csb = S0.sb("csb", [128, 8, 36]); bcsb = Buf()
        sel = S0.sb("sel", [64, 8, 128]); bsel = Buf()
        cmask = S0.sb("cmask", [128, 2, 64]); bcm = Buf()
        k.dma("sp", sel[:], C.D["csel"], writes=[bsel])
        k.dma("sp", cmask[:], C.D["cmask"], writes=[bcm])
        Sg = Scope(C)
        Sg.__enter__()
        preT = Sg.sb("preT", [16, T_ALL]); bpre = Buf()
        with Scope(C) as S1:
            uT = S1.sb("uT", [128, 8, T_ALL], BF16); buT = Buf()
            norm_phase(C, L, uT, buT)
            W = Ctx()
            W.wst = [S1.sb("wst", [128, 8, 128]) for _ in range(2)]; W.bwst = [Buf(), Buf()]
            W.w = [S1.sb("wbf", [128, 8, 128], BF16) for _ in range(2)]; W.bw = [Buf(), Buf()]
            bank = [S1.ps("bank", [128, 512]) for _ in range(8)]; bbank = [Buf() for _ in range(8)]
            bdt = S1.sb("bdt", [128, 48, 128]); bbdt = Buf()
            gst = S1.sb("gst", [128, 48, 16]); bgst = Buf()
            Gqk = S1.sb("Gqk", [128, 16, 16], BF16); Gv = S1.sb("Gv", [128, 16, 16], BF16); bG = Buf()
            xl = S1.sb("xl", [128, T_LAT + 3]); xcx = S1.sb("xcx", [128, T_CTX + 3]); bxl = Buf()
            acc = S1.sb("acc", [128, T_LAT]); bacc = Buf()
            xmb = S1.sb("xmb", [128, T_ALL], BF16); bxmb = Buf()
            xcb = S1.sb("xcb", [128, T_ALL], BF16); bxcb = Buf()
            szs = [S1.sb("szs", [128, 512]) for _ in range(2)]; bszs = [Buf(), Buf()]
            wog = S1.sb("wog", [128, 8, 512]); bwog = Buf()
            wogb = S1.sb("wogb", [128, 8, 512], BF16); bwogb = Buf()
            for a in range(3):
                k.dma("sp", bdt[:, a * 16:(a + 1) * 16, :], C.D["cBDT"][a].rearrange("f p q -> p f q"), writes=[bbdt])
                k.dma("sp", gst[:, a * 16:(a + 1) * 16, :], C.D["cG"][a].rearrange("f p g -> p f g"), writes=[bgst])
            for fc in range(16):
                mm(C, bank[2][:, 0:16], bdt[:, fc, :], gst[:, fc, :], True, False, [bbdt, bgst], [bbank[2]])
                mm(C, bank[2][:, 0:16], bdt[:, 16 + fc, :], gst[:, 16 + fc, :], False, True, [bbdt, bgst], [bbank[2]])
                mm(C, bank[2][:, 16:32], bdt[:, 32 + fc, :], gst[:, 32 + fc, :], True, True, [bbdt, bgst], [bbank[2]])
                act(C, Gqk[:, fc, :], bank[2][:, 0:16], AF.Copy, [bbank[2]], [bG])
                act(C, Gv[:, fc, :], bank[2][:, 16:32], AF.Copy, [bbank[2]], [bG])
            k.op("pool", lambda e: e.memset(xl[:], 0.0), [], [bxl])
            k.op("pool", lambda e: e.memset(xcx[:], 0.0), [], [bxl])
            for fc in range(int(os.environ.get("MG_FC", "16"))):
                load_w_slices(C, W, w_in, [fc * 128, 2048 + fc * 128])
                for ci, (c0, n) in enumerate(TCH):
                    pj, bpj = bank[ci % 2], bbank[ci % 2]
                    for kk in range(8):
                        mm(C, pj[:, :n], W.w[0][:, kk, :], uT[:, kk, c0:c0 + n], kk == 0, kk == 7, [W.bw[0], buT], [bpj])
                    dst = xl[:, 2 + c0:2 + c0 + n] if c0 < T_LAT else xcx[:, 2:2 + n]
                    act(C, dst, pj[:, :n], AF.Copy, [bpj], [bxl])
                    k.op("dve", lambda e: e.tensor_copy(xmb[:, c0:c0 + n], dst), [bxl], [bxmb])
                SK = os.environ.get("MG_SKIP", "")
                cw = lambda jx: vcol(C, "c_convw", 1, fc * 4 + jx)
                for (src, T, o0) in (() if "c" in SK else ((xl, T_LAT, 0), (xcx, T_CTX, T_LAT))):
                    k.op("dve", lambda e: e.tensor_scalar(acc[:, :T], src[:, 0:T], cw(0), None, op0=ALU.mult), [bxl, C.bvecs], [bacc])
                    for jx in range(1, 4):
                        k.op("dve", lambda e: e.scalar_tensor_tensor(out=acc[:, :T], in0=src[:, jx:jx + T], scalar=cw(jx), in1=acc[:, :T],
                                                                     op0=ALU.mult, op1=ALU.add), [bxl, C.bvecs, bacc], [bacc])
                    act(C, xcb[:, o0:o0 + T], acc[:, :T], AF.Silu, [bacc, C.bvecs], [bxcb], bias=vcol(C, "c_convb", 1, fc), scale=1.0)
                qd = os.environ.get("DMAQ", "pool")
                if qd != "none":
                    k.dma(qd, xmT_d[fc], xmb[:], reads=[bxmb], writes=[bxm[fc]])
                    k.dma(qd, xcT_d[fc], xcb[:], reads=[bxcb], writes=[bxc[fc]])
                for ci, (c0, n) in enumerate(() if "g" in SK else TCH):
                    mm(C, bank[3 + ci][0:16, :n], Gqk[:, fc, :], xcb[:, c0:c0 + n], fc == 0, False, [bG, bxcb], [bbank[3 + ci]])
                    mm(C, bank[3 + ci][0:16, :n], Gv[:, fc, :], xmb[:, c0:c0 + n], False, fc == 15, [bG, bxmb], [bbank[3 + ci]])
                for ci, (c0, n) in enumerate(() if "z" in SK else TCH):
                    pj, bpj = bank[ci % 2], bbank[ci % 2]
                    for kk in range(8):
                        mm(C, pj[:, :n], W.w[1][:, kk, :], uT[:, kk, c0:c0 + n], kk == 0, kk == 7, [W.bw[1], buT], [bpj])
                    b = ci % 2
                    act(C, szs[b][:, :n], pj[:, :n], AF.Silu, [bpj], [bszs[b]])
                    k.dma("pool", szT_d[fc][:, c0:c0 + n], szs[b][:, :n], reads=[bszs[b]], writes=[bsz[fc]])
            for nb in range(int(os.environ.get("MG_OG", "4"))):
                k.dma("sp", wog[:], w_in[:, 4096 + nb * 512:4096 + (nb + 1) * 512].rearrange("(k p) n -> p k n", p=128), writes=[bwog])
                k.op("pool", lambda e: e.tensor_copy(wogb[:], wog[:]), [bwog], [bwogb])
                for i in range(NT):
                    pj, bpj = bank[i % 2], bbank[i % 2]
                    b = i % 2
                    for kk in range(8):
                        mm(C, pj[:, :], uT[:, kk, i * 128:(i + 1) * 128], wogb[:, kk, :], kk == 0, kk == 7, [bwogb, buT], [bpj])
                    act(C, szs[b][:, :], pj[:, :], AF.Sigmoid, [bpj], [bszs[b]])
                    k.dma("pool", sog_d[i * 128:(i + 1) * 128, nb * 512:(nb + 1) * 512], szs[b][:, :], reads=[bszs[b]], writes=[bsog])
            for ci, (c0, n) in enumerate(TCH):
                act(C, preT[:, c0:c0 + n], bank[3 + ci][0:16, :n], AF.Copy, [bbank[3 + ci]], [bpre])
        MS = int(os.environ.get("MS", "9"))
        if MS <= 2 and "dbg_pre" not in C.D:
            C.D["dbg_pre"] = nc.dram_tensor("dbg_pre", [16, T_ALL], F32, kind="ExternalOutput").ap()
            C.D["dbg_qt"] = nc.dram_tensor("dbg_qt", [128, NT * 4 * 36], F32, kind="ExternalOutput").ap()
            C.D["dbg_nar"] = nc.dram_tensor("dbg_nar", [36, T_ALL], F32, kind="ExternalOutput").ap()
            C.D["dbg_csb"] = nc.dram_tensor("dbg_csb", [128, 8 * 36], F32, kind="ExternalOutput").ap()
        if MS <= 2:
            k.dma("sp", C.D["dbg_pre"], preT[:], reads=[bpre])
        with Scope(C) as S2:
            GI = S2.sb("GI", [64, T_ALL]); GF = S2.sb("GF", [64, T_ALL]); Bt = S2.sb("Bt", [64, T_ALL])
            Aa = S2.sb("Aa", [64, T_ALL]); TM = S2.sb("TM", [64, T_ALL]); PV = S2.sb("PV", [64, T_ALL])
            ON = S2.sb("ON", [64, T_ALL])
            gb = S2.sb("gb", [64, 4]); cs = S2.sb("cs", [64, 36])
            bg = Buf()
            ps_t = S2.ps("ps_t", [128, 512]); bps = Buf()
            R = slice(0, 36)
            for t in (GI, GF, TM):
                k.op("pool", lambda e: e.memset(t[:], 0.0), [], [bg])
            k.op("pool", lambda e: e.memset(ON[:], 1.0), [], [bg])
            k.dma("sp", gb[:, 0:2], C.D["cgb"], writes=[bg])
            k.dma("sp", GI[0:4, 256:T_ALL], preT[0:4, 0:T_LAT], reads=[bpre], writes=[bg])
            k.dma("sp", GI[0:4, 0:256], preT[0:4, T_LAT:T_ALL], reads=[bpre], writes=[bg])
            k.dma("sp", GF[0:4, 256:T_ALL], preT[4:8, 0:T_LAT], reads=[bpre], writes=[bg])
            k.dma("sp", GF[0:4, 0:256], preT[4:8, T_LAT:T_ALL], reads=[bpre], writes=[bg])
            k.dma("sp", TM[32:36, :], preT[8:12, :], reads=[bpre], writes=[bg])
            k.dma("sp", PV[32:36, :], preT[12:16, :], reads=[bpre], writes=[bg])
            k.barrier()
            k.op("dve", lambda e: e.tensor_copy(GI[32:36, ::-1], TM[32:36, :]), [bg], [bg])
            k.op("dve", lambda e: e.tensor_copy(GF[32:36, ::-1], PV[32:36, :]), [bg], [bg])
            k.op("dve", lambda e: e.tensor_scalar(gb[:, 2:3], gb[:, 1:2], -1.0, None, op0=ALU.mult), [bg], [bg])
            k.op("dve", lambda e: e.tensor_scalar(GI[R, :], GI[R, :], gb[R, 0:1], None, op0=ALU.add), [bg], [bg])
            act(C, GF[R, :], GF[R, :], AF.Exp, [bg], [bg], bias=gb[R, 2:3], scale=-1.0)
            act(C, GF[R, :], GF[R, :], AF.Ln, [bg, C.bconst], [bg], bias=C.ones_f[R, 0:1], scale=1.0)
            k.op("dve", lambda e: e.tensor_scalar(GF[R, :], GF[R, :], -1.0, None, op0=ALU.mult), [bg], [bg])
            k.op("dve", lambda e: e.tensor_tensor_scan(Bt[R, :], ON[R, :], GF[R, :], 0.0, ALU.mult, ALU.add), [bg], [bg])
            k.op("dve", lambda e: e.tensor_tensor(out=Aa[R, :], in0=GI[R, :], in1=Bt[R, :], op=ALU.subtract), [bg], [bg])
            k.op("dve", lambda e: e.tensor_tensor_scan(NAr[R, :], ON[R, :], Aa[R, :], 0.0, ALU.mult, ALU.max), [bg], [bg, bNAr])
            A3 = NAr[R, :].rearrange("p (c s) -> p c s", s=64)
            a3 = Aa[R, :].rearrange("p (c s) -> p c s", s=64)
            P3 = PV[R, :].rearrange("p (c s) -> p c s", s=64)
            T3 = TM[R, :].rearrange("p (c s) -> p c s", s=64)
            k.op("pool", lambda e: e.memset(PV[:], 0.0), [bg], [bg])
            k.op("dve", lambda e: e.tensor_copy(P3[:, 1:36, :], A3[:, 0:35, 63:64].broadcast_to([36, 35, 64])), [bg, bNAr], [bg])
            k.op("dve", lambda e: e.tensor_tensor(out=T3, in0=a3, in1=A3[:, :, 63:64].broadcast_to([36, 36, 64]), op=ALU.subtract), [bg, bNAr], [bg])
            act(C, GF[R, :], TM[R, :], AF.Exp, [bg], [bg])
            k.op("dve", lambda e: e.tensor_tensor(out=TM[R, :], in0=PV[R, :], in1=NAr[R, :], op=ALU.subtract), [bg, bNAr], [bg])
            act(C, GI[R, :], TM[R, :], AF.Exp, [bg], [bg])
            k.op("dve", lambda e: e.tensor_tensor(out=cs[R, :], in0=P3[:, :, 0], in1=A3[:, :, 63], op=ALU.subtract), [bg, bNAr], [bg])
            act(C, cs[R, :], cs[R, :], AF.Exp, [bg], [bg])
            k.op("dve", lambda e: e.tensor_tensor(out=Bt[R, :], in0=Bt[R, :], in1=NAr[R, :], op=ALU.add), [bg, bNAr], [bg])
            act(C, Bt[R, :], Bt[R, :], AF.Exp, [bg], [bg], scale=-1.0)
            k.op("dve", lambda e: e.tensor_scalar(NAr[R, :], NAr[R, :], -1.0, None, op0=ALU.mult), [bg, bNAr], [bg, bNAr])
            for t in (Aa, GF, GI, Bt, NAr):
                k.op("dve", lambda e: e.tensor_copy(TM[32:36, ::-1], t[32:36, :]), [bg, bNAr], [bg])
                k.op("dve", lambda e: e.tensor_copy(t[32:36, :], TM[32:36, :]), [bg], [bg, bNAr])
            for bl in range(NT):
                for q, t in enumerate((Aa, GF, GI, Bt)):
                    C.k.op("pe", lambda e: e.transpose(ps_t[:, q * 36:(q + 1) * 36], t[R, bl * 128:(bl + 1) * 128], C.ident_f[R, 0:36]),
                           [bg, C.bconst], [bps], pe_acc=True)
                act(C, QT[:, bl, :, :].rearrange("p q r -> p (q r)"), ps_t[:, 0:144], AF.Copy, [bps], [bQT])
            for ch in range(8):
                mm(C, ps_t[:, 0:36], sel[R, ch, :], cs[R, :], True, True, [bsel, bg], [bps])
                act(C, csb[:, ch, :], ps_t[:, 0:36], AF.Copy, [bps], [bcsb])
        if MS <= 2:
            k.dma("sp", C.D["dbg_qt"], QT[:].rearrange("p a b c -> p (a b c)"), reads=[bQT])
            k.dma("sp", C.D["dbg_nar"], NAr[0:36, :], reads=[bNAr])
            k.dma("sp", C.D["dbg_csb"], csb[:].rearrange("p a b -> p (a b)"), reads=[bcsb])
        Sg.__exit__(None, None, None)
        for hd in range(4 if MS > 2 else 0):
            with Scope(C) as S3:
                hbuf = S3.sb("hbuf", [128, NT, 512]); bhb = [Buf() for _ in range(NT * 2)]
                with Scope(C) as Sa:
                    mlstm_scan_head(C, Sa, hd, hbuf, bhb, QT, bQT, NAr, bNAr, csb, bcsb, sel, bsel, cmask, bcm, xmT_d, xcT_d, bxm, bxc)
                with Scope(C) as Sb:
                    mlstm_finish_head(C, Sb, L, hd, hbuf, bhb, xcT_d, szT_d, sog_d, bxc, bsz, bsog, w_out)


def mlstm_scan_head(C, S, hd, hbuf, bhb, QT, bQT, NAr, bNAr, csb, bcsb, sel, bsel, cmask, bcm, xmT_d, xcT_d, bxm, bxc):
    k = C.k
    qT = S.sb("qTh", [128, 4, T_ALL], BF16); bq = Buf()
    kT = S.sb("kTh", [128, 4, T_ALL], BF16); bk_ = Buf()
    vt = S.sb("vtok", [128, NT, 512], BF16); bvt = Buf()
    xs0 = S.sb("xs", [128, T_ALL], BF16); xs = [xs0, xs0]; bxs0 = Buf(); bxs = [bxs0, bxs0]
    xm0 = S.sb("xms", [128, T_ALL], BF16); xm = [xm0, xm0]; bxm0 = Buf(); bxms = [bxm0, bxm0]
    bdf = S.sb("bdf", [128, 12, 128]); bbdf = Buf()
    bdb = S.sb("bdb", [128, 12, 128], BF16); bbdb = Buf()
    NAbc = [S.sb("NAbc", [128, T_ALL]) for _ in range(2)]; bNA = [Buf(), Buf()]
    Cst = [S.sb("Cst", [128, 4, 512]) for _ in range(2)]; bC = [Buf(), Buf()]
    Cbf = [S.sb("Cbf", [128, 4, 512], BF16) for _ in range(2)]; bCb = [Buf(), Buf()]
    nst = [S.sb("nst", [128, 4]) for _ in range(2)]; nbf = [S.sb("nbf", [128, 4, 2], BF16) for _ in range(2)]
    bn_ = [Buf(), Buf()]; bnb = [Buf(), Buf()]
    Dm = [S.sb("Dm", [128, 64]) for _ in range(2)]; bDm = [Buf(), Buf()]
    WT = [S.sb("WT", [128, 64], BF16) for _ in range(2)]; bWT = [Buf(), Buf()]
    dd = [S.sb("dd", [128, 8]) for _ in range(2)]; bdd = [Buf(), Buf()]
    t1 = [S.sb("t1", [128, 512]) for _ in range(2)]; bt1 = [Buf(), Buf()]
    kd = [S.sb("kd", [128, 512], BF16) for _ in range(2)]; bkd = [Buf(), Buf()]
    bank = [S.ps("bank", [128, 512]) for _ in range(7)]; bbank = [Buf() for _ in range(7)]
    bkt = S.ps("bkt", [128, 1024], BF16); bbkt = Buf()
    for a in range(3):
        k.dma("sp", bdf[:, a * 4:(a + 1) * 4, :], C.D["cBD"][a, 4 * hd:4 * hd + 4].rearrange("f p q -> p f q"), writes=[bbdf])
    k.op("dve", lambda e: e.tensor_copy(bdb[:], bdf[:]), [bbdf], [bbdb])
    for c in range(4):
        fc = 4 * hd + c
        b = c % 2
        k.dma("sp", xs[b][:], xcT_d[fc], reads=[bxc[fc]], writes=[bxs[b]])
        k.dma("sp", xm[b][:], xmT_d[fc], reads=[bxm[fc]], writes=[bxms[b]])
        for ci, (c0, n) in enumerate(TCH):
            pj, bpj = bank[ci % 2], bbank[ci % 2]
            mm(C, pj[:, :n], bdb[:, c, :], xs[b][:, c0:c0 + n], True, True, [bbdb, bxs[b]], [bpj])
            act(C, qT[:, c, c0:c0 + n], pj[:, :n], AF.Copy, [bpj], [bq])
            pj, bpj = bank[2 + ci % 2], bbank[2 + ci % 2]
            mm(C, pj[:, :n], bdb[:, 4 + c, :], xs[b][:, c0:c0 + n], True, True, [bbdb, bxs[b]], [bpj])
            act(C, kT[:, c, c0:c0 + n], pj[:, :n], AF.Identity, [bpj], [bk_], scale=512.0 ** -0.5)
        for g in range(5):
            tiles = list(range(4 * g, min(4 * g + 4, NT)))
            pj, bpj = bank[4 + g % 2], bbank[4 + g % 2]
            for s, i in enumerate(tiles):
                mm(C, pj[:, s * 128:(s + 1) * 128], xm[b][:, i * 128:(i + 1) * 128], bdb[:, 8 + c, :], True, True, [bbdb, bxms[b]], [bpj])
            n = len(tiles)
            act(C, vt[:, 4 * g:4 * g + n, c * 128:(c + 1) * 128], pj[:, :n * 128].rearrange("p (s e) -> p s e", e=128), AF.Copy, [bpj], [bvt])
    for d in range(2):
        ch = d * 4 + hd
        for ci, (c0, n) in enumerate(TCH):
            pj, bpj = bank[ci % 2], bbank[ci % 2]
            mm(C, pj[:, :n], sel[0:36, ch, :], NAr[0:36, c0:c0 + n], True, True, [bsel, bNAr], [bpj])
            act(C, NAbc[d][:, c0:c0 + n], pj[:, :n], AF.Copy, [bpj], [bNA[d]])
        k.op("pool", lambda e: e.memset(Cst[d][:], 0.0), [], [bC[d]])
        k.op("pool", lambda e: e.memset(Cbf[d][:], 0.0), [], [bCb[d]])
        k.op("pool", lambda e: e.memset(nst[d][:], 0.0), [], [bn_[d]])
        k.op("pool", lambda e: e.memset(nbf[d][:], 0.0), [], [bnb[d]])
    written = set()
    for st in range(36):
        for d in range(2):
            ch = d * 4 + hd
            r = d * 32 + hd
            if d == 0:
                c0 = 64 * st
                tok0 = T_LAT + 64 * st if st < 4 else 64 * (st - 4)
            else:
                tok0 = T_LAT + 64 * (3 - st) if st < 4 else 64 * (35 - st)
                c0 = tok0
            i = tok0 // 128
            p = (tok0 % 128) // 64
            hs = slice(p * 64, (p + 1) * 64)
            bl = c0 // 128
            col = lambda q: QT[hs, bl, q, r:r + 1]
            bs, bbs = bank[3 * d], bbank[3 * d]
            bn, bbn = bank[3 * d + 1], bbank[3 * d + 1]
            bn2, bbn2 = bank[3 * d + 2], bbank[3 * d + 2]
            bh_ = bhb[2 * i + p]
            tk = slice(tok0, tok0 + 64)
            for dc in range(4):
                mm(C, bs[hs, 0:64], kT[:, dc, tk], qT[:, dc, tk], dc == 0, dc == 3, [bk_, bq], [bbs])
            k.op("dve", lambda e: e.tensor_scalar(Dm[d][hs, :], NAbc[d][hs, c0:c0 + 64], col(0), None, op0=ALU.add),
                 [bNA[d], bQT], [bDm[d]])
            k.op("dve", lambda e: e.tensor_scalar_min(Dm[d][hs, :], Dm[d][hs, :], 0.0), [bDm[d]], [bDm[d]])
            act(C, Dm[d][hs, :], Dm[d][hs, :], AF.Exp, [bDm[d]], [bDm[d]])
            k.op("dve", lambda e: e.tensor_tensor(out=Dm[d][hs, :], in0=Dm[d][hs, :], in1=cmask[hs, d, :], op=ALU.mult), [bDm[d], bcm], [bDm[d]])
            k.op("dve", lambda e: e.tensor_tensor(out=WT[d][hs, :], in0=bs[hs, 0:64], in1=Dm[d][hs, :], op=ALU.mult), [bbs, bDm[d]], [bWT[d]])
            mm(C, bn[hs, :], WT[d][hs, :], vt[hs, i, :], True, True, [bWT[d], bvt], [bbn])
            for dc in range(4):
                mm(C, bn2[hs, :], qT[:, dc, tk], Cbf[d][:, dc, :], dc == 0, dc == 3, [bq, bCb[d]], [bbn2])
            mm(C, bs[hs, 64:65], WT[d][hs, :], C.ones_b[hs, 0:1], True, True, [bWT[d], C.bconst], [bbs])
            for dc in range(4):
                mm(C, bs[hs, 65:66], qT[:, dc, tk], nbf[d][:, dc, 0:1], dc == 0, dc == 3, [bq, bnb[d]], [bbs])
            D_ = dd[d]
            k.op("dve", lambda e: e.tensor_tensor(out=D_[hs, 0:1], in0=bs[hs, 65:66], in1=col(2), op=ALU.mult), [bbs, bQT], [bdd[d]])
            k.op("dve", lambda e: e.tensor_tensor(out=D_[hs, 1:2], in0=bs[hs, 64:65], in1=D_[hs, 0:1], op=ALU.add), [bbs, bdd[d]], [bdd[d]])
            k.op("dve", lambda e: e.tensor_scalar(D_[hs, 5:6], D_[hs, 1:2], -1.0, None, op0=ALU.mult), [bdd[d]], [bdd[d]])
            k.op("dve", lambda e: e.tensor_tensor(out=D_[hs, 5:6], in0=D_[hs, 5:6], in1=D_[hs, 1:2], op=ALU.max), [bdd[d]], [bdd[d]])
            k.op("dve", lambda e: e.tensor_tensor(out=D_[hs, 2:3], in0=D_[hs, 5:6], in1=col(3), op=ALU.max), [bdd[d], bQT], [bdd[d]])
            k.op("dve", lambda e: e.reciprocal(D_[hs, 3:4], D_[hs, 2:3]), [bdd[d]], [bdd[d]])
            k.op("dve", lambda e: e.tensor_tensor(out=D_[hs, 4:5], in0=D_[hs, 3:4], in1=col(2), op=ALU.mult), [bdd[d], bQT], [bdd[d]])
            if (i, p) not in written:
                written.add((i, p))
                k.op("dve", lambda e: e.tensor_scalar(t1[d][hs, :], bn2[hs, :], D_[hs, 4:5], None, op0=ALU.mult), [bbn2, bdd[d]], [bt1[d]])
            else:
                k.op("dve", lambda e: e.scalar_tensor_tensor(out=t1[d][hs, :], in0=bn2[hs, :], scalar=D_[hs, 4:5], in1=hbuf[hs, i, :],
                                                             op0=ALU.mult, op1=ALU.add), [bbn2, bdd[d], bh_], [bt1[d]])
            k.op("dve", lambda e: e.scalar_tensor_tensor(out=hbuf[hs, i, :], in0=bn[hs, :], scalar=D_[hs, 3:4], in1=t1[d][hs, :],
                                                         op0=ALU.mult, op1=ALU.add), [bbn, bdd[d], bt1[d]], [bh_])
            if st == 35:
                continue
            for dc in range(4):
                C.k.op("pe", lambda e: e.transpose(bkt[hs, dc * 128:(dc + 1) * 128], kT[:, dc, tk], C.ident_b[:]), [bk_, C.bconst], [bbkt], pe_acc=True)
            k.op("dve", lambda e: e.tensor_scalar(kd[d][hs, :], bkt[hs, 0:512], col(1), None, op0=ALU.mult), [bbkt, bQT], [bkd[d]])
            cscol = csb[:, ch, st:st + 1]
            for dc in range(4):
                mm(C, bs[:, 66 + dc:67 + dc], kd[d][hs, dc * 128:(dc + 1) * 128], C.ones_b[hs, 0:1], True, True, [bkd[d], C.bconst], [bbs])
            k.op("dve", lambda e: e.scalar_tensor_tensor(out=nst[d][:], in0=nst[d][:], scalar=cscol, in1=bs[:, 66:70], op0=ALU.mult, op1=ALU.add),
                 [bn_[d], bcsb, bbs], [bn_[d]])
            k.op("dve", lambda e: e.tensor_copy(nbf[d][:, :, 0], nst[d][:]), [bn_[d]], [bnb[d]])
            for dc in range(4):
                bd_, bbd_ = bank[6], bbank[6]
                mm(C, bd_[:, :], kd[d][hs, dc * 128:(dc + 1) * 128], vt[hs, i, :], True, True, [bkd[d], bvt], [bbd_])
                k.op("dve", lambda e: e.scalar_tensor_tensor(out=Cst[d][:, dc, :], in0=Cst[d][:, dc, :], scalar=cscol, in1=bd_[:, :],
                                                             op0=ALU.mult, op1=ALU.add), [bC[d], bcsb, bbd_], [bC[d]])
                act(C, Cbf[d][:, dc, :], Cst[d][:, dc, :], AF.Copy, [bC[d]], [bCb[d]])


def mlstm_finish_head(C, S, L, hd, hbuf, bhb, xcT_d, szT_d, sog_d, bxc, bsz, bsog, w_out):
    k = C.k
    alloc_outproj_bufs(C, S)
    yT4 = S.sb("yT4", [128, 4, T_ALL], BF16); byT = Buf()
    sg = [S.sb("sg", [128, 512]) for _ in range(2)]; bsg = [Buf(), Buf()]
    ho = [S.sb("ho", [128, 512]) for _ in range(2)]; bho = [Buf(), Buf()]
    junk = S.sb("junk", [128, 512]); bjunk = Buf()
    st = S.sb("st", [128, 4, 8]); bst = [Buf() for _ in range(4)]
    hnb = [S.sb("hnb", [128, 512], BF16) for _ in range(4)]; bhn = [Buf() for _ in range(4)]
    xcg = [S.sb("xcg", [128, 512], BF16) for _ in range(2)]; bxcg = [Buf(), Buf()]
    szg = [S.sb("szg", [128, 512]) for _ in range(2)]; bszg = [Buf(), Buf()]
    u1 = [S.sb("u1", [128, 512]) for _ in range(2)]; bu1 = [Buf(), Buf()]
    tp = [S.ps("tp", [128, 8, 128], BF16) for _ in range(2)]; btp = [Buf(), Buf()]
    bank = [S.ps("bank", [128, 512]) for _ in range(2)]; bbank = [Buf(), Buf()]
    it = 0
    for g in range(5):
        tiles = list(range(4 * g, min(4 * g + 4, NT)))
        n = len(tiles)
        for s, i in enumerate(tiles):
            b = i % 2
            k.dma("sp", sg[b][:], sog_d[i * 128:(i + 1) * 128, hd * 512:(hd + 1) * 512], reads=[bsog], writes=[bsg[b]])
            k.op("dve", lambda e: e.tensor_tensor(out=ho[b][:], in0=hbuf[:, i, :], in1=sg[b][:], op=ALU.mult),
                 [bhb[2 * i], bhb[2 * i + 1], bsg[b]], [bho[b]])
            k.op("pool", lambda e: e.memset(st[:, s, 0:2], 0.0), [], [bst[s]])
            act(C, junk[:], ho[b][:], AF.Identity, [bho[b]], [bjunk, bst[s]], accum_out=st[:, s, 0:1])
            k.op("dve", lambda e: e.tensor_scalar(st[:, s, 2:3], st[:, s, 0:1], -1.0 / 512, None, op0=ALU.mult), [bst[s]], [bst[s]])
            act(C, ho[b][:], ho[b][:], AF.Identity, [bho[b], bst[s]], [bho[b]], bias=st[:, s, 2:3], scale=1.0)
            act(C, junk[:], ho[b][:], AF.Square, [bho[b]], [bjunk, bst[s]], accum_out=st[:, s, 1:2])
            k.op("dve", lambda e: e.tensor_scalar(st[:, s, 3:4], st[:, s, 1:2], 1.0 / 512, EPS, op0=ALU.mult, op1=ALU.add), [bst[s]], [bst[s]])
            act(C, st[:, s, 4:5], st[:, s, 3:4], AF.Sqrt, [bst[s]], [bst[s]])
            k.op("dve", lambda e: e.reciprocal(st[:, s, 5:6], st[:, s, 4:5]), [bst[s]], [bst[s]])
            k.op("dve", lambda e: e.tensor_scalar(hnb[s][:], ho[b][:], st[:, s, 5:6], None, op0=ALU.mult), [bho[b], bst[s]], [bhn[s]])
        t0 = tiles[0] * 128
        for c in range(4):
            fc = 4 * hd + c
            b = it % 2
            it += 1
            for s, i in enumerate(tiles):
                C.k.op("pe", lambda e: e.transpose(tp[b][:, s, :], hnb[s][:, c * 128:(c + 1) * 128], C.ident_b[:]), [bhn[s], C.bconst], [btp[b]], pe_acc=True)
            k.dma("sp", xcg[b][:, :n * 128], xcT_d[fc][:, t0:t0 + n * 128], reads=[bxc[fc]], writes=[bxcg[b]])
            k.dma("sp", szg[b][:, :n * 128], szT_d[fc][:, t0:t0 + n * 128], reads=[bsz[fc]], writes=[bszg[b]])
            k.op("dve", lambda e: e.tensor_scalar(u1[b][:, :n * 128], tp[b][:, 0:n, :].rearrange("p s t -> p (s t)"), vcol(C, "c_mhn", 1, fc), None, op0=ALU.mult),
                 [btp[b], C.bvecs], [bu1[b]])
            k.op("dve", lambda e: e.scalar_tensor_tensor(out=u1[b][:, :n * 128], in0=xcg[b][:, :n * 128], scalar=vcol(C, "c_skip", 1, fc), in1=u1[b][:, :n * 128],
                                                         op0=ALU.mult, op1=ALU.add), [bxcg[b], C.bvecs, bu1[b]], [bu1[b]])
            k.op("dve", lambda e: e.tensor_tensor(out=yT4[:, c, t0:t0 + n * 128], in0=u1[b][:, :n * 128], in1=szg[b][:, :n * 128], op=ALU.mult),
                 [bu1[b], bszg[b]], [byT])
    outproj_pass(C, L, S, yT4, byT, w_out, 4 * hd, hd == 0, hd == 3, bank, bbank)


LAYER_FN[2] = mlstm_layer


_PROG = {}


SAMPLES_PER_CORE = 4


def kernel(**inputs):
    per = prep_inputs(inputs)
    n = len(per)
    S = SAMPLES_PER_CORE
    ncores = n // S
    if "p" not in _PROG:
        _PROG["p"] = build_program(layers=(0, 1, 2, 3), debug=False, S=S)
    nc, C = _PROG["p"]
    in_maps = []
    for c in range(ncores):
        m = {}
        for name in C.in_names:
            if name[:-1] in ("x", "ctx", "vecs") and name[-1].isdigit():
                m[name] = per[c * S + int(name[-1])][name[:-1]]
            else:
                m[name] = per[0][name]
        in_maps.append(m)
    res = run_bass_kernel_spmd(nc, in_maps, core_ids=list(range(ncores)))
    out = np.stack([np.asarray(res.results[b // S][f"out{b % S}"], dtype=np.float32) for b in range(n)], axis=0)
    return out
```
